# Optimizing a Trainium2 kernel written in Bass

```python
import jax, jax.numpy as jnp
from jax import lax
import numpy as np

D_MODEL = 1024
BATCH = 4
SEQ = 8192
DEPTH = 2

GRID_W = 64
CTX_LEN = 256
N_MIXERS = 2
N_EVEN = (DEPTH + 1) // 2
N_ODD = DEPTH // 2

CHUNK = 128
A_WIDTH = D_MODEL
A_GROUPS = 16
A_GROUP_CH = A_WIDTH // A_GROUPS

HEAD_DIM = 64
N_Q_HEADS = D_MODEL // HEAD_DIM
N_KV_HEADS = 4
Q_PER_KV = N_Q_HEADS // N_KV_HEADS
QKV_WIDTH = (N_Q_HEADS + 2 * N_KV_HEADS) * HEAD_DIM
WINDOW = 128
BLOCK = 128
ROPE_BASE = 10000.0

D_FF = (7 * D_MODEL) // 2
N_EXPERTS = 8
TOP_K = 2

EPS = 1e-6
NEG_INF = -1e30

kernel_name = 'hybrid_chunkmlp_swa_moe_dit'


def rms_norm(x, g):
    xf = x.astype(jnp.float32)
    y = xf * lax.rsqrt(jnp.mean(xf * xf, axis=-1, keepdims=True) + EPS)
    return (y * g.astype(jnp.float32)).astype(x.dtype)


def modulate(x, shift, scale):
    return x * (1 + scale) + shift


def rope_1d(x, pos):
    half = x.shape[-1] // 2
    inv = ROPE_BASE ** (-jnp.arange(half, dtype=jnp.float32) / half)
    ang = pos.astype(jnp.float32)[:, None] * inv[None, :]
    cos = jnp.cos(ang)[:, None, :]
    sin = jnp.sin(ang)[:, None, :]
    xf = x.astype(jnp.float32)
    x1, x2 = xf[..., :half], xf[..., half:]
    return jnp.concatenate([x1 * cos - x2 * sin, x1 * sin + x2 * cos], axis=-1).astype(x.dtype)


def axial_rope(x, row, col):
    half = x.shape[-1] // 2
    return jnp.concatenate([rope_1d(x[..., :half], row), rope_1d(x[..., half:], col)], axis=-1)


def chunk_token_mlp(x, w_in, v_g, v_b, w_s, b_s, w_out):
    B, S, _ = x.shape
    uv = jax.nn.gelu(x @ w_in)
    u, v = uv[..., :A_WIDTH], uv[..., A_WIDTH:]
    vf = v.astype(jnp.float32)
    mu = jnp.mean(vf, axis=-1, keepdims=True)
    var = jnp.mean(jnp.square(vf - mu), axis=-1, keepdims=True)
    v = ((vf - mu) * lax.rsqrt(var + EPS) * v_g.astype(jnp.float32) + v_b.astype(jnp.float32)).astype(x.dtype)
    v = v.reshape(B, S // CHUNK, CHUNK, A_GROUPS, A_GROUP_CH)
    s = jnp.einsum('gpq,bnqgc->bnpgc', w_s, v) + b_s.T[None, None, :, :, None]
    return (u * s.reshape(B, S, A_WIDTH)) @ w_out


def split_qkv(y):
    B, S, _ = y.shape
    nq = N_Q_HEADS * HEAD_DIM
    nk = N_KV_HEADS * HEAD_DIM
    q = y[..., :nq].reshape(B, S, N_Q_HEADS, HEAD_DIM)
    k = y[..., nq:nq + nk].reshape(B, S, N_KV_HEADS, HEAD_DIM)
    v = y[..., nq + nk:].reshape(B, S, N_KV_HEADS, HEAD_DIM)
    return q, k, v


def window_attention(hn, zn, w_qkv, sink, w_out, with_ctx_out):
    B, S, _ = hn.shape
    L = zn.shape[1]
    rows = S // GRID_W
    row = jnp.repeat(jnp.arange(rows, dtype=jnp.int32), GRID_W)
    col = jnp.tile(jnp.arange(GRID_W, dtype=jnp.int32), rows)
    q, k, v = split_qkv(hn @ w_qkv)
    qc, kc, vc = split_qkv(zn @ w_qkv)
    q = axial_rope(q, row, col)
    k = axial_rope(k, row, col)
    scale = HEAD_DIM ** -0.5
    nb = S // BLOCK
    kw = BLOCK + 2 * WINDOW
    kp = jnp.pad(k, ((0, 0), (WINDOW, WINDOW), (0, 0), (0, 0)))
    vp = jnp.pad(v, ((0, 0), (WINDOW, WINDOW), (0, 0), (0, 0)))
    qs = jnp.moveaxis(q.reshape(B, nb, BLOCK, N_KV_HEADS, Q_PER_KV, HEAD_DIM), 1, 0)
    sink_l = sink.astype(jnp.float32).reshape(N_KV_HEADS, Q_PER_KV)[None, :, :, None, None]
    q_idx = jnp.arange(BLOCK)[:, None]
    k_rel = jnp.arange(kw)[None, :] - WINDOW
    band = jnp.abs(q_idx - k_rel) <= WINDOW

    def attend_block(args):
        n, qb = args
        kb = lax.dynamic_slice_in_dim(kp, n * BLOCK, kw, axis=1)
        vb = lax.dynamic_slice_in_dim(vp, n * BLOCK, kw, axis=1)
        kpos = n * BLOCK + k_rel
        valid = band & (kpos >= 0) & (kpos < S)
        s_loc = jnp.einsum('bqhgd,bkhd->bhgqk', qb, kb).astype(jnp.float32) * scale
        s_loc = jnp.where(valid, s_loc, NEG_INF)
        s_ctx = jnp.einsum('bqhgd,bkhd->bhgqk', qb, kc).astype(jnp.float32) * scale
        s_sink = jnp.broadcast_to(sink_l, s_loc.shape[:-1] + (1,))
        p = jax.nn.softmax(jnp.concatenate([s_loc, s_ctx, s_sink], axis=-1), axis=-1).astype(qb.dtype)
        return (jnp.einsum('bhgqk,bkhd->bqhgd', p[..., :kw], vb)
                + jnp.einsum('bhgqk,bkhd->bqhgd', p[..., kw:kw + L], vc))

    o = lax.map(attend_block, (jnp.arange(nb, dtype=jnp.int32), qs))
    y = jnp.moveaxis(o, 0, 1).reshape(B, S, N_Q_HEADS * HEAD_DIM) @ w_out
    if not with_ctx_out:
        return y, None
    qcg = qc.reshape(B, L, N_KV_HEADS, Q_PER_KV, HEAD_DIM)
    s_c = jnp.einsum('bqhgd,bkhd->bhgqk', qcg, kc).astype(jnp.float32) * scale
    s_cs = jnp.broadcast_to(sink_l, s_c.shape[:-1] + (1,))
    p_c = jax.nn.softmax(jnp.concatenate([s_c, s_cs], axis=-1), axis=-1).astype(qc.dtype)
    o_c = jnp.einsum('bhgqk,bkhd->bqhgd', p_c[..., :L], vc)
    yc = o_c.reshape(B, L, N_Q_HEADS * HEAD_DIM) @ w_out
    return y, yc


def swiglu(x, w_gate, w_up, w_down):
    return (jax.nn.silu(x @ w_gate) * (x @ w_up)) @ w_down


def moe_swiglu(x, w_router, w_gate, w_up, w_down):
    B, S, D = x.shape
    xt = x.reshape(-1, D)
    logits = (xt @ w_router).astype(jnp.float32)
    top_v, top_i = lax.top_k(logits, TOP_K)
    top_w = jax.nn.softmax(top_v, axis=-1)
    gates = jnp.sum(jax.nn.one_hot(top_i, N_EXPERTS, dtype=jnp.float32) * top_w[..., None], axis=1)
    gates = gates.astype(x.dtype)
    y = jnp.zeros_like(xt)
    for e in range(N_EXPERTS):
        y = y + gates[:, e:e + 1] * swiglu(xt, w_gate[e], w_up[e], w_down[e])
    return y.reshape(B, S, D)


def setup_inputs(seed: int = 0) -> dict:
    key = jax.random.key(seed)
    ks = jax.random.split(key, 24)
    f32 = jnp.float32
    D = D_MODEL
    nrm = lambda k, shape, s: jax.random.normal(k, shape, f32) * s
    return {
        'x': nrm(ks[0], (BATCH, SEQ, D), 1.0),
        'c': nrm(ks[1], (BATCH, D), 1.0),
        'ctx': nrm(ks[2], (BATCH, CTX_LEN, D), 1.0),
        'c_ctx': nrm(ks[3], (D,), 1.0),
        'ada_w': nrm(ks[4], (DEPTH, D, 6 * D), 0.5 * D ** -0.5),
        'ada_b': nrm(ks[5], (DEPTH, 6 * D), 0.01),
        'norm_g': 1.0 + nrm(ks[6], (DEPTH, 2, D), 0.02),
        'final_g': 1.0 + nrm(ks[7], (D,), 0.02),
        'a_w_in': nrm(ks[8], (N_EVEN, D, 2 * A_WIDTH), D ** -0.5),
        'a_v_g': 1.0 + nrm(ks[9], (N_EVEN, A_WIDTH), 0.02),
        'a_v_b': nrm(ks[10], (N_EVEN, A_WIDTH), 0.02),
        'a_w_s': nrm(ks[11], (N_EVEN, A_GROUPS, CHUNK, CHUNK), CHUNK ** -0.5),
        'a_b_s': 1.0 + nrm(ks[12], (N_EVEN, A_GROUPS, CHUNK), 0.02),
        'a_w_out': nrm(ks[13], (N_EVEN, A_WIDTH, D), A_WIDTH ** -0.5),
        'b_w_qkv': nrm(ks[14], (N_ODD, D, QKV_WIDTH), D ** -0.5),
        'b_sink': nrm(ks[15], (N_ODD, N_Q_HEADS), 0.5),
        'b_w_out': nrm(ks[16], (N_ODD, N_Q_HEADS * HEAD_DIM, D), (N_Q_HEADS * HEAD_DIM) ** -0.5),
        'ffn_w_gate': nrm(ks[17], (N_EVEN, D, D_FF), D ** -0.5),
        'ffn_w_up': nrm(ks[18], (N_EVEN, D, D_FF), D ** -0.5),
        'ffn_w_down': nrm(ks[19], (N_EVEN, D_FF, D), D_FF ** -0.5),
        'moe_w_router': nrm(ks[20], (N_ODD, D, N_EXPERTS), D ** -0.5),
        'moe_w_gate': nrm(ks[21], (N_ODD, N_EXPERTS, D, D_FF), D ** -0.5),
        'moe_w_up': nrm(ks[22], (N_ODD, N_EXPERTS, D, D_FF), D ** -0.5),
        'moe_w_down': nrm(ks[23], (N_ODD, N_EXPERTS, D_FF, D), D_FF ** -0.5),
    }


def reference(x, c, ctx, c_ctx, ada_w, ada_b, norm_g, final_g,
              a_w_in, a_v_g, a_v_b, a_w_s, a_b_s, a_w_out,
              b_w_qkv, b_sink, b_w_out,
              ffn_w_gate, ffn_w_up, ffn_w_down,
              moe_w_router, moe_w_gate, moe_w_up, moe_w_down):
    h = x
    z = ctx
    ada_in = jax.nn.silu(c)
    ada_in_ctx = jax.nn.silu(c_ctx)[None]
    for i in range(DEPTH):
        j = i // 2
        last = i == DEPTH - 1
        mod = (ada_in @ ada_w[i] + ada_b[i])[:, None, :]
        mod_c = (ada_in_ctx @ ada_w[i] + ada_b[i])[:, None, :]
        sh1, sc1, g1, sh2, sc2, g2 = jnp.split(mod, 6, axis=-1)
        sh1c, sc1c, g1c, sh2c, sc2c, g2c = jnp.split(mod_c, 6, axis=-1)
        hn = modulate(rms_norm(h, norm_g[i, 0]), sh1, sc1)
        zn = modulate(rms_norm(z, norm_g[i, 0]), sh1c, sc1c)
        if i % N_MIXERS == 0:
            dh = chunk_token_mlp(hn, a_w_in[j], a_v_g[j], a_v_b[j], a_w_s[j], a_b_s[j], a_w_out[j])
            dz = None if last else chunk_token_mlp(zn, a_w_in[j], a_v_g[j], a_v_b[j], a_w_s[j], a_b_s[j], a_w_out[j])
        else:
            dh, dz = window_attention(hn, zn, b_w_qkv[j], b_sink[j], b_w_out[j], not last)
        if i % 2 == 0:
            channel = lambda t: swiglu(t, ffn_w_gate[j], ffn_w_up[j], ffn_w_down[j])
        else:
            channel = lambda t: moe_swiglu(t, moe_w_router[j], moe_w_gate[j], moe_w_up[j], moe_w_down[j])
        h = h + g1 * dh
        h = h + g2 * channel(modulate(rms_norm(h, norm_g[i, 1]), sh2, sc2))
        if not last:
            z = z + g1c * dz
            z = z + g2c * channel(modulate(rms_norm(z, norm_g[i, 1]), sh2c, sc2c))
    return rms_norm(h, final_g)
```

```python
from contextlib import ExitStack
import numpy as np
import concourse.bass as bass
import concourse.mybir as mybir
from concourse.bass_utils import run_bass_kernel_spmd

F32 = mybir.dt.float32
BF16 = mybir.dt.bfloat16
AF = mybir.ActivationFunctionType
ALU = mybir.AluOpType
AX = mybir.AxisListType

D = 1024
DFF = 3584
NHS = 7
NE = 8
SEQ = 8192
OWN = 4096
NCH = 36
EPS = 1e-6


class DSem:
    def __init__(self, fw, name):
        self.sem = fw.new_sem(name)
        self.count = 0
        self.key = "d:%s:%d" % (name, fw._nsem)


class Buf:
    def __init__(self, name, dsem=None):
        self.name = name
        self.dsem = dsem
        self.last_w = None
        self.readers = {}


class FW:
    def __init__(self, nc):
        self.nc = nc
        self._stack = []
        self.eng = {"pe": nc.tensor, "act": nc.scalar, "dve": nc.vector,
                    "pool": nc.gpsimd, "sp": nc.sync}
        self.sem = {}
        self.cnt = {}
        for k in ("pe", "act", "dve", "pool"):
            self.sem[k] = self.new_sem("e_" + k)
            self.cnt[k] = 0
        self.waited = {k: {} for k in self.eng}
        self.dsems = []

    def new_sem(self, name):
        self._nsem = getattr(self, "_nsem", 0) + 1
        cm = self.nc.semaphore("%s_%d" % (name, self._nsem))
        s = cm.__enter__()
        self._stack.append(cm)
        return s

    def dsem(self, name, async_=False):
        d = DSem(self, name)
        d.async_ = async_
        self.dsems.append(d)
        return d

    def buf(self, name, dsem=None):
        return Buf(name, dsem)

    def bufs(self, name, n, dsem=None):
        return [Buf("%s%d" % (name, i), dsem) for i in range(n)]

    def _wait(self, eng, tok):
        if tok is None:
            return
        sem, val, key = tok[:3]
        if len(tok) > 3:
            val = tok[3].count
            tok[3].waited_since_issue = True
        if self.waited[eng].get(key, 0) >= val:
            return
        self.eng[eng].wait_ge(sem, val)
        self.waited[eng][key] = val

    def _deps(self, eng, reads, writes):
        for b in reads:
            t = b.last_w
            if t is not None:
                if t[2] == eng and eng == "pe":
                    continue
                self._wait(eng, t)
        for b in writes:
            t = b.last_w
            if t is not None and t[2] != eng:
                self._wait(eng, t)
            for k, t in b.readers.items():
                if k != eng:
                    self._wait(eng, t)

    def _commit(self, tok, reads, writes):
        for b in reads:
            b.readers[tok[2]] = tok
        for b in writes:
            b.last_w = tok
            b.readers = {}

    def op(self, eng, fn, reads=(), writes=()):
        self._deps(eng, reads, writes)
        ins = fn(self.eng[eng])
        self.cnt[eng] += 1
        ins.then_inc(self.sem[eng], 1)
        tok = (self.sem[eng], self.cnt[eng], eng)
        self._commit(tok, reads, writes)
        return ins

    def dma(self, q, out, in_, reads=(), writes=(), dsem=None, **kw):
        if dsem is None:
            dsem = writes[0].dsem
        assert dsem is not None
        self._deps(q, reads, writes)
        if getattr(dsem, "waited_since_issue", False) and dsem.count > 0:
            self._wait(q, (dsem.sem, dsem.count, dsem.key))
        dsem.waited_since_issue = False
        ins = self.eng[q].dma_start(out=out, in_=in_, **kw)
        dsem.count += 16
        ins.then_inc(dsem.sem, 16)
        tok = (dsem.sem, dsem.count, dsem.key, dsem)
        self._commit(tok, reads, writes)
        return ins

    def idma(self, out, out_off, in_, in_off, reads=(), writes=(), dsem=None):
        q = "pool"
        if dsem is None:
            dsem = writes[0].dsem
        self._deps(q, reads, writes)
        if getattr(dsem, "waited_since_issue", False) and dsem.count > 0:
            self._wait(q, (dsem.sem, dsem.count, dsem.key))
        dsem.waited_since_issue = False
        ins = self.nc.gpsimd.indirect_dma_start(out=out, out_offset=out_off, in_=in_, in_offset=in_off)
        dsem.count += 16
        ins.then_inc(dsem.sem, 16)
        tok = (dsem.sem, dsem.count, dsem.key, dsem)
        self._commit(tok, reads, writes)
        return ins

    def barrier(self):
        toks = [(self.sem[k], self.cnt[k], k) for k in self.sem if self.cnt[k] > 0]
        toks += [(d.sem, d.count, d.key) for d in self.dsems if d.count > 0 and not d.async_]
        for e in self.eng:
            for t in toks:
                if t[2] != e:
                    self._wait(e, t)

    def finish(self, eng="sp"):
        toks = [(d.sem, d.count, d.key) for d in self.dsems if d.count > 0]
        toks += [(self.sem[k], self.cnt[k], k) for k in self.sem if self.cnt[k] > 0]
        for t in toks:
            self._wait(eng, t)


def build(stop_after=99, debug=False):
    nc = bass.Bass("TRN2", target_bir_lowering=False)
    fw = FW(nc)

    def din(name, shape, dt=F32):
        return nc.dram_tensor(name, list(shape), dt, kind="ExternalInput").ap()

    def dscr(name, shape, dt=F32):
        kind = "ExternalOutput" if (debug and dt == F32) else "Internal"
        return nc.dram_tensor(name, list(shape), dt, kind=kind).ap()

    x_loc = din("x_loc", [OWN + 256, D])
    ctx_b = din("ctx_b", [256, D])
    c2 = din("c2", [128, 16])
    ada_w = din("ada_w", [2, D, 6 * D])
    ada_bT = din("ada_bT", [128, 2, 48])
    ada_b = din("ada_b", [2, 6 * D])
    norm_gT = din("norm_gT", [128, 32])
    norm_g_raw = din("norm_g_raw", [2, 2, D])
    tri_d = din("tri", [128, 128])
    final_g = din("final_g", [D])
    a_w_in = din("a_w_in", [D, 2 * D])
    a_v_g = din("a_v_g", [D])
    a_v_b = din("a_v_b", [D])
    a_w_s = din("a_w_s", [16, 128, 128])
    a_b_sT = din("a_b_sT", [128, 16])
    a_w_out = din("a_w_out", [D, D])
    b_w_qkv = din("b_w_qkv", [D, 1536])
    b_sink = din("b_sink", [16])
    b_w_out = din("b_w_out", [D, D])
    ffn_wg = din("ffn_w_gate", [D, DFF])
    ffn_wu = din("ffn_w_up", [D, DFF])
    ffn_wd = din("ffn_w_down", [DFF, D])
    if stop_after >= 4:
        moe_wr = din("moe_w_router", [D, NE])
        moe_wg = din("moe_w_gate", [NE, D, DFF])
        moe_wu = din("moe_w_up", [NE, D, DFF])
        moe_wd = din("moe_w_down", [NE, DFF, D])
    ident_d = din("ident", [128, 128])
    cos_d = din("cos_t", [64, NCH * 128])
    sin_d = din("sin_t", [64, NCH * 128])
    masks_d = din("masks", [128, 4, 128])

    out_d = nc.dram_tensor("out", [OWN, D], F32, kind="ExternalOutput").ap()

    hA = dscr("hA", [NCH * 128, D])
    h1 = dscr("h1", [NCH * 128, D])
    hB = dscr("hB", [OWN, D])
    gb_d = dscr("gb_d", [8, 128, D])
    w_in_b = dscr("w_in_b", [D, 2 * D], BF16)
    w_aout_b = dscr("w_aout_b", [D, D], BF16)
    w_qkv_b = dscr("w_qkv_b", [D, 1536], BF16)
    w_bout_b = dscr("w_bout_b", [D, D], BF16)
    wg_b = dscr("wg_b", [1, NHS, 128, 8 * 512], BF16)
    wu_b = dscr("wu_b", [1, NHS, 128, 8 * 512], BF16)
    wd_b = dscr("wd_b", [1, NHS, 128, 4 * D], BF16)
    wgud = dscr("wgud", [NE * NHS * 128, 3 * 4096], BF16)
    NSLOT = 24 * 512
    XS = dscr("XS", [NSLOT, D], BF16)
    YS = nc.dram_tensor("YS", [NSLOT, D], F32, kind="Internal").ap()
    d_XS = fw.dsem("XS"); b_XS = fw.buf("XS", d_XS)
    d_YS = fw.dsem("YS"); b_YS = fw.buf("YS", d_YS)

    d_out = fw.dsem("out")
    b_out = fw.buf("out", d_out)
    d_hA = fw.dsem("hA"); b_hA = fw.buf("hA", d_hA)
    d_h1 = fw.dsem("h1"); b_h1 = fw.buf("h1", d_h1)
    d_hB = fw.dsem("hB"); b_hB = fw.buf("hB", d_hB)
    d_gb = fw.dsem("gb"); b_gb = fw.buf("gb", d_gb)
    d_w0 = fw.dsem("w0", True); b_w0 = fw.buf("w0", d_w0)
    d_w1 = fw.dsem("w1", True); b_w1 = fw.buf("w1", d_w1)
    d_wf = [fw.dsem("wf%d" % e, True) for e in range(1 + NE)]
    b_wf = [fw.buf("wf%d" % e, d_wf[e]) for e in range(1 + NE)]

    cast_jobs = []

    def cast(dst, src, b):
        cast_jobs.append((dst, src, b))

    def pump_casts(n):
        for _ in range(n):
            if cast_jobs:
                dst, src, b = cast_jobs.pop(0)
                fw.dma("pool", dst, src, writes=[b])

    for j in range(4):
        cast(w_in_b[j * 256:(j + 1) * 256, :], a_w_in[j * 256:(j + 1) * 256, :], b_w0)
    for j in range(2):
        cast(w_aout_b[j * 512:(j + 1) * 512, :], a_w_out[j * 512:(j + 1) * 512, :], b_w0)

    def cast_ffn(e, wg, wu, wd):
        for hs in range(NHS):
            cast(wg_b[e, hs].rearrange("p (k n) -> p k n", k=8),
                 wg[:, hs * 512:(hs + 1) * 512].rearrange("(k p) n -> p k n", p=128), b_wf[e])
            cast(wu_b[e, hs].rearrange("p (k n) -> p k n", k=8),
                 wu[:, hs * 512:(hs + 1) * 512].rearrange("(k p) n -> p k n", p=128), b_wf[e])
            cast(wd_b[e, hs].rearrange("p (f n) -> p f n", f=4),
                 wd[hs * 512:(hs + 1) * 512, :].rearrange("(f p) n -> p f n", p=128), b_wf[e])

    pump_casts(len(cast_jobs))
    if stop_after >= 2:
        cast_ffn(0, ffn_wg, ffn_wu, ffn_wd)
    if stop_after >= 3:
        for j in range(4):
            cast(w_qkv_b[j * 256:(j + 1) * 256, :], b_w_qkv[j * 256:(j + 1) * 256, :], b_w1)
        for j in range(2):
            cast(w_bout_b[j * 512:(j + 1) * 512, :], b_w_out[j * 512:(j + 1) * 512, :], b_w1)
    if stop_after >= 4:
        for e in range(NE):
            for hs in range(NHS):
                rows = wgud[(e * NHS + hs) * 128:(e * NHS + hs + 1) * 128, :]
                cast(rows[:, 0:4096].rearrange("p (k n) -> p k n", k=8),
                     moe_wg[e][:, hs * 512:(hs + 1) * 512].rearrange("(k p) n -> p k n", p=128), b_wf[1 + e])
                cast(rows[:, 4096:8192].rearrange("p (k n) -> p k n", k=8),
                     moe_wu[e][:, hs * 512:(hs + 1) * 512].rearrange("(k p) n -> p k n", p=128), b_wf[1 + e])
                cast(rows[:, 8192:12288].rearrange("p (f n) -> p f n", f=4),
                     moe_wd[e][hs * 512:(hs + 1) * 512, :].rearrange("(f p) n -> p f n", p=128), b_wf[1 + e])

    gst = ExitStack()
    ps = [gst.enter_context(nc.psum_tensor("ps%d" % i, [128, 512], F32)) for i in range(8)]
    b_ps = fw.bufs("ps", 8)

    nuid = [0]

    def S(es, name, shape, dt):
        nuid[0] += 1
        return es.enter_context(nc.sbuf_tensor("s%d_%s" % (nuid[0], name), list(shape), dt))

    d_c = fw.dsem("const")
    ident = S(gst, "ident", [128, 128], F32); b_ident = fw.buf("ident", d_c)
    fw.dma("sp", ident[:], ident_d, writes=[b_ident])
    modT = S(gst, "modT", [128, 2, 48, 2], F32); b_modT = fw.buf("modT")
    scl = S(gst, "scl", [128, 2, 2, 2, 8], F32); b_scl = fw.buf("scl")
    eps_t = S(gst, "eps_t", [128, 1], F32); b_eps = fw.buf("eps")
    fw.op("dve", lambda e: e.memset(eps_t[:], EPS), writes=[b_eps])
    ss2 = [S(gst, "ss%d" % i, [128, 4], F32) for i in range(2)]; b_ss2 = fw.bufs("ss", 2)
    junk = S(gst, "junk", [128, D], BF16); b_junk = fw.buf("junk")
    xs2 = [S(gst, "xs%d" % i, [128, D], F32) for i in range(2)]; b_xs2 = fw.bufs("xs", 2)
    ss, b_ss = ss2[0], b_ss2[0]
    nnt = [0]

    with ExitStack() as es:
        cs = S(es, "cs", [128, 8, 2], F32); b_cs = fw.buf("cs", d_c)
        fw.dma("sp", cs[:].rearrange("p k j -> p (k j)"), c2, writes=[b_cs])
        csil = S(es, "csil", [128, 8, 2], F32); b_csil = fw.buf("csil")
        fw.op("act", lambda e: e.activation(csil[:], cs[:], AF.Silu), reads=[b_cs], writes=[b_csil])
        crep = S(es, "crep", [128, 2, 8, 128], F32); b_crep = fw.buf("crep")
        for j in range(2):
            fw.op("dve", lambda e: e.tensor_copy(crep[:, j], csil[:, :, j:j + 1].to_broadcast([128, 8, 128])),
                  reads=[b_csil], writes=[b_crep])
        abT = S(es, "abT", [128, 2, 48], F32); b_abT = fw.buf("abT", d_c)
        fw.dma("sp", abT[:], ada_bT, writes=[b_abT])
        ngT = S(es, "ngT", [128, 2, 2, 8], F32); b_ngT = fw.buf("ngT", d_c)
        fw.dma("sp", ngT[:].rearrange("p a b k -> p (a b k)"), norm_gT, writes=[b_ngT])
        d_slab = [fw.dsem("aw0"), fw.dsem("aw1")]
        slabs = [S(es, "awslab%d" % i, [128, 8, D], F32) for i in range(2)]
        b_slab = [fw.buf("awslab%d" % i, d_slab[i]) for i in range(2)]
        d_bb = fw.dsem("bb")
        bb = S(es, "bb", [128, D], F32); b_bb = fw.buf("bb", d_bb)
        gtmp = S(es, "gtmp", [128, D], F32); b_gtmp = fw.buf("gtmp")
        n = 0
        for i in range(2):
            for v in range(6):
                sl, bsl = slabs[n % 2], b_slab[n % 2]
                n += 1
                for k in range(8):
                    fw.dma("sp", sl[:, k, :], ada_w[i, k * 128:(k + 1) * 128, v * D:(v + 1) * D], writes=[bsl])
                for m in range(8):
                    for k in range(8):
                        fw.op("pe", lambda e: e.matmul(ps[0][:, m * 2:m * 2 + 2], lhsT=sl[:, k, m * 128:(m + 1) * 128],
                                                       rhs=csil[:, k, :], start=(k == 0), stop=(k == 7)),
                              reads=[bsl, b_csil], writes=[b_ps[0]])
                fw.op("dve", lambda e: e.tensor_tensor(
                    modT[:, i, v * 8:(v + 1) * 8, :], ps[0][:, 0:16].rearrange("p (m j) -> p m j", j=2),
                    abT[:, i, v * 8:(v + 1) * 8].unsqueeze(2).to_broadcast([128, 8, 2]), ALU.add),
                    reads=[b_ps[0], b_abT], writes=[b_modT])
                if v in (2, 5) or (i == 1 and v in (3, 4)):
                    fw.dma("sp", bb[:], ada_b[i, v * D:(v + 1) * D].partition_broadcast(128), writes=[b_bb])
                    for j in range(2):
                        if (i == 1 and j == 1) or (v in (3, 4) and j == 1):
                            continue
                        for s in range(2):
                            for k in range(8):
                                fw.op("pe", lambda e: e.matmul(ps[1 + s][:], lhsT=crep[:, j, k, :],
                                                               rhs=sl[:, k, s * 512:(s + 1) * 512],
                                                               start=(k == 0), stop=(k == 7)),
                                      reads=[bsl, b_crep], writes=[b_ps[1 + s]])
                            fw.op("dve", lambda e: e.tensor_tensor(gtmp[:, s * 512:(s + 1) * 512], ps[1 + s][:],
                                                                   bb[:, s * 512:(s + 1) * 512], ALU.add),
                                  reads=[b_ps[1 + s], b_bb], writes=[b_gtmp])
                        gi = {(0, 2, 0): 0, (0, 5, 0): 1, (0, 2, 1): 2, (0, 5, 1): 3, (1, 2, 0): 4, (1, 5, 0): 5,
                              (1, 3, 0): 6, (1, 4, 0): 7}[(i, v, j)]
                        fw.dma("sp", gb_d[gi], gtmp[:], reads=[b_gtmp], writes=[b_gb])
        for i in range(2):
            for nn in range(2):
                scv = 1 + 3 * nn
                for j in range(2):
                    fw.op("dve", lambda e: e.scalar_tensor_tensor(
                        scl[:, i, nn, j, :], modT[:, i, scv * 8:(scv + 1) * 8, j], 1.0, ngT[:, i, nn, :],
                        ALU.add, ALU.mult), reads=[b_modT, b_ngT], writes=[b_scl])
        fw.barrier()

    def shift_ap(i, nn, j, k):
        shv = 3 * nn
        return modT[:, i, shv * 8 + k, j:j + 1]

    def norm_transpose(hx_ap, b_hx, i, nn, j, dst_fn, b_dst, pb=(0, 1), pb_alt=None):
        par = nnt[0] % 2
        nnt[0] += 1
        ss, b_ss, xs, b_xs = ss2[par], b_ss2[par], xs2[par], b_xs2[par]
        if pb_alt is not None and par == 1:
            pb = pb_alt
        fw.op("dve", lambda e: e.memset(ss[:, 0:1], 0.0), writes=[b_ss])
        fw.op("act", lambda e: e.activation(junk[:], hx_ap, AF.Square, accum_out=ss[:, 0:1]),
              reads=[b_hx], writes=[b_ss])
        fw.op("act", lambda e: e.activation(ss[:, 1:2], ss[:, 0:1], AF.Sqrt, bias=eps_t[:, 0:1], scale=1.0 / D),
              reads=[b_ss, b_eps], writes=[b_ss])
        fw.op("dve", lambda e: e.reciprocal(ss[:, 2:3], ss[:, 1:2]), reads=[b_ss], writes=[b_ss])
        fw.op("dve", lambda e: e.tensor_scalar(xs[:], hx_ap, ss[:, 2:3], None, ALU.mult),
              reads=[b_hx, b_ss], writes=[b_xs])
        halves = [range(0, 8)] if pb[0] != pb[1] else [range(0, 4), range(4, 8)]
        for ks in halves:
            for k in ks:
                bank = pb[k // 4]
                fw.op("pe", lambda e: e.transpose(ps[bank][:, (k % 4) * 128:(k % 4 + 1) * 128],
                                                  xs[:, k * 128:(k + 1) * 128], ident[:]),
                      reads=[b_xs, b_ident], writes=[b_ps[bank]])
            for k in ks:
                bank = pb[k // 4]
                fw.op("act", lambda e: e.activation(dst_fn(k), ps[bank][:, (k % 4) * 128:(k % 4 + 1) * 128],
                                                    AF.Identity, bias=shift_ap(i, nn, j, k), scale=scl[:, i, nn, j, k:k + 1]),
                      reads=[b_ps[bank], b_scl, b_modT], writes=[b_dst])
        return xs, b_xs

    def chunk_src(ci):
        if ci < 34:
            return x_loc[ci * 128:(ci + 1) * 128, :]
        return ctx_b[(ci - 34) * 128:(ci - 33) * 128, :]

    def modset(ci):
        return 1 if ci >= 34 else 0

    if stop_after >= 1:
        with ExitStack() as es:
            d_p1 = fw.dsem("p1w")
            wu_sb = S(es, "wu_sb", [128, 8, D], BF16)
            wv_sb = S(es, "wv_sb", [128, 8, D], BF16)
            wo_sb = S(es, "wo_sb", [128, 8, D], BF16)
            b_wsb = fw.buf("p1w", d_p1)
            for k in range(8):
                fw.dma("sp", wu_sb[:, k, :], w_in_b[k * 128:(k + 1) * 128, 0:D], reads=[b_w0], writes=[b_wsb])
                fw.dma("sp", wv_sb[:, k, :], w_in_b[k * 128:(k + 1) * 128, D:2 * D], reads=[b_w0], writes=[b_wsb])
                fw.dma("sp", wo_sb[:, k, :], w_aout_b[k * 128:(k + 1) * 128, :], reads=[b_w0], writes=[b_wsb])
            wsp = S(es, "wsp", [128, 16, 128], F32); b_wsp = fw.buf("wsp", d_p1)
            fw.dma("sp", wsp[:], a_w_s.rearrange("g p q -> p g q"), writes=[b_wsp])
            bsT = S(es, "bsT", [128, 16], F32); b_bs = fw.buf("bs", d_p1)
            fw.dma("sp", bsT[:], a_b_sT, writes=[b_bs])
            vg_bc = S(es, "vg_bc", [128, D], F32); vb_bc = S(es, "vb_bc", [128, D], F32)
            b_vgb = fw.buf("vgb", d_p1)
            fw.dma("sp", vg_bc[:], a_v_g.partition_broadcast(128), writes=[b_vgb])
            fw.dma("sp", vb_bc[:], a_v_b.partition_broadcast(128), writes=[b_vgb])
            g1 = S(es, "g1", [128, 2, D], F32); b_g1 = fw.buf("g1", d_p1)
            fw.dma("sp", g1[:, 0, :], gb_d[0], reads=[b_gb], writes=[b_g1])
            fw.dma("sp", g1[:, 1, :], gb_d[2], reads=[b_gb], writes=[b_g1])
            wsT = S(es, "wsT", [128, 16, 128], BF16); b_wsT = fw.buf("wsT")
            for g in range(16):
                bk = g % 2
                fw.op("pe", lambda e: e.transpose(ps[bk][:, 0:128], wsp[:, g, :], ident[:]),
                      reads=[b_wsp, b_ident], writes=[b_ps[bk]])
                fw.op("act", lambda e: e.copy(wsT[:, g, :], ps[bk][:, 0:128]), reads=[b_ps[bk]], writes=[b_wsT])
            idb1 = S(es, "idb1", [128, 128], BF16); b_idb1 = fw.buf("idb1")
            fw.op("dve", lambda e: e.tensor_copy(idb1[:], ident[:]), reads=[b_ident], writes=[b_idb1])

            d_hx = [fw.dsem("hx0"), fw.dsem("hx1")]
            hx = [S(es, "hx%d" % i, [128, 4, D], F32) for i in range(2)]
            b_hx = [[fw.buf("hx%d_%d" % (i, c), d_hx[i]) for c in range(4)] for i in range(2)]
            xnT = S(es, "xnT", [128, 8, 512], BF16); b_xnT = fw.bufs("xnT", 4)
            uf2 = [S(es, "uf%d" % i, [128, D], F32) for i in range(2)]; b_uf2 = fw.bufs("uf", 2)
            vf2 = [S(es, "vf%d" % i, [128, D], F32) for i in range(2)]; b_vf2 = fw.bufs("vf", 2)
            vbf2 = [S(es, "vbf%d" % i, [128, D], BF16) for i in range(2)]; b_vbf2 = fw.bufs("vbf", 2)
            bn2 = [S(es, "bn%d" % i, [128, 2, 6], F32) for i in range(2)]
            mv2 = [S(es, "mv%d" % i, [128, 4], F32) for i in range(2)]; b_bn2 = fw.bufs("bn", 2)
            stmp2 = [S(es, "stmp%d" % i, [128, 512], F32) for i in range(2)]; b_stmp2 = fw.bufs("stmp", 2)
            ttok2 = [S(es, "ttok%d" % i, [128, D], BF16) for i in range(2)]; b_ttok2 = fw.bufs("ttok", 2)
            tT82 = [S(es, "tT8_%d" % i, [128, 8, 128], BF16) for i in range(2)]; b_tT82 = fw.bufs("tT8", 2)
            otmp2 = [S(es, "otmp%d" % i, [128, 512], F32) for i in range(2)]; b_otmp2 = fw.bufs("otmp", 2)
            psb0 = ps[0][:].bitcast(BF16)

            tiles = [list(range(t * 4, t * 4 + 4)) for t in range(8)] + [[32, 33], [34, 35]]

            def load_tile1(ti):
                if ti >= len(tiles):
                    return
                for c, ci in enumerate(tiles[ti]):
                    fw.dma("sp", hx[ti % 2][:, c, :], chunk_src(ci), writes=[b_hx[ti % 2][c]])

            xnT_1b = S(es, "xnT1b", [128, 8, 512], BF16); b_xnT_1b = fw.bufs("xnT1b", 4)
            xn1 = [(xnT, b_xnT), (xnT_1b, b_xnT_1b)]

            def norm_tile1(ti_):
                if ti_ >= len(tiles):
                    return
                xn_, bxn_ = xn1[ti_ % 2]
                for c_, ci_ in enumerate(tiles[ti_]):
                    norm_transpose(hx[ti_ % 2][:, c_, :], b_hx[ti_ % 2][c_], 0, 0, modset(tiles[ti_][0]),
                                   lambda k: xn_[:, k, c_ * 128:(c_ + 1) * 128], bxn_[c_])

            load_tile1(0)
            norm_tile1(0)
            for ti, chunks in enumerate(tiles):
                nt = len(chunks) * 128
                hb, bhb = hx[ti % 2], b_hx[ti % 2]
                j = modset(chunks[0])
                load_tile1(ti + 1)
                pump_casts(6)
                xnT, b_xnT = xn1[ti % 2]

                def stage_a(c):
                    b2 = c % 2
                    for (w_, dstt, b_dstt, pbase) in ((wu_sb, uf2[b2], b_uf2[b2], 2), (wv_sb, vf2[b2], b_vf2[b2], 4)):
                        for s in range(2):
                            for k in range(8):
                                fw.op("pe", lambda e: e.matmul(ps[pbase + s][:], lhsT=xnT[:, k, c * 128:(c + 1) * 128],
                                                               rhs=w_[:, k, s * 512:(s + 1) * 512],
                                                               start=(k == 0), stop=(k == 7)),
                                      reads=[b_wsb, b_xnT[c]], writes=[b_ps[pbase + s]])
                            fw.op("act", lambda e: e.activation(dstt[:, s * 512:(s + 1) * 512], ps[pbase + s][:], AF.Gelu_apprx_tanh),
                                  reads=[b_ps[pbase + s]], writes=[b_dstt])
                    for s in range(2):
                        fw.op("dve", lambda e: e.bn_stats(bn2[b2][:, s, :], vf2[b2][:, s * 512:(s + 1) * 512]),
                              reads=[b_vf2[b2]], writes=[b_bn2[b2]])
                    fw.op("dve", lambda e: e.bn_aggr(mv2[b2][:, 0:2], bn2[b2][:]), reads=[b_bn2[b2]], writes=[b_bn2[b2]])
                    fw.op("act", lambda e: e.activation(mv2[b2][:, 2:3], mv2[b2][:, 1:2], AF.Sqrt, bias=eps_t[:, 0:1], scale=1.0),
                          reads=[b_bn2[b2], b_eps], writes=[b_bn2[b2]])
                    fw.op("dve", lambda e: e.reciprocal(mv2[b2][:, 3:4], mv2[b2][:, 2:3]), reads=[b_bn2[b2]], writes=[b_bn2[b2]])
                    fw.op("dve", lambda e: e.tensor_scalar(vf2[b2][:], vf2[b2][:], mv2[b2][:, 0:1], mv2[b2][:, 3:4],
                                                           ALU.subtract, ALU.mult),
                          reads=[b_vf2[b2], b_bn2[b2]], writes=[b_vf2[b2]])
                    fw.op("pool", lambda e: e.tensor_tensor(vf2[b2][:], vf2[b2][:], vg_bc[:], ALU.mult),
                          reads=[b_vf2[b2], b_vgb], writes=[b_vf2[b2]])
                    fw.op("pool", lambda e: e.tensor_tensor(vbf2[b2][:], vf2[b2][:], vb_bc[:], ALU.add),
                          reads=[b_vf2[b2], b_vgb], writes=[b_vbf2[b2]])

                def stage_b1(c):
                    b2 = c % 2
                    for g in range(16):
                        bk = 6 + g // 8
                        fw.op("pe", lambda e: e.matmul(ps[bk][:, (g % 8) * 64:(g % 8 + 1) * 64], lhsT=wsT[:, g, :],
                                                       rhs=vbf2[b2][:, g * 64:(g + 1) * 64], start=True, stop=True),
                              reads=[b_vbf2[b2], b_wsT], writes=[b_ps[bk]])
                    for hf in range(2):
                        bk = 6 + hf
                        fw.op("dve", lambda e: e.tensor_tensor(
                            stmp2[hf][:].rearrange("p (g c) -> p g c", g=8), ps[bk][:].rearrange("p (g c) -> p g c", g=8),
                            bsT[:, hf * 8:(hf + 1) * 8].unsqueeze(2).to_broadcast([128, 8, 64]), ALU.add),
                            reads=[b_ps[bk], b_bs], writes=[b_stmp2[hf]])
                        fw.op("dve", lambda e: e.tensor_tensor(ttok2[b2][:, hf * 512:(hf + 1) * 512], stmp2[hf][:],
                                                               uf2[b2][:, hf * 512:(hf + 1) * 512], ALU.mult),
                              reads=[b_stmp2[hf], b_uf2[b2]], writes=[b_ttok2[b2]])
                    for k in range(8):
                        fw.op("pe", lambda e: e.transpose(psb0[:, k * 128:(k + 1) * 128], ttok2[b2][:, k * 128:(k + 1) * 128], idb1[:]),
                              reads=[b_ttok2[b2], b_idb1], writes=[b_ps[0]])
                    fw.op("act", lambda e: e.copy(tT82[b2][:].rearrange("p k q -> p (k q)"), psb0[:, :]),
                          reads=[b_ps[0]], writes=[b_tT82[b2]])

                def stage_b2(c, ci):
                    b2 = c % 2
                    for s in range(2):
                        bk = (1, 3)[s]
                        for k in range(8):
                            fw.op("pe", lambda e: e.matmul(ps[bk][:], lhsT=tT82[b2][:, k, :],
                                                           rhs=wo_sb[:, k, s * 512:(s + 1) * 512],
                                                           start=(k == 0), stop=(k == 7)),
                                  reads=[b_tT82[b2], b_wsb], writes=[b_ps[bk]])
                        fw.op("dve", lambda e: e.tensor_tensor(otmp2[s][:], ps[bk][:], g1[:, j, s * 512:(s + 1) * 512], ALU.mult),
                              reads=[b_ps[bk], b_g1], writes=[b_otmp2[s]])
                        fw.op("pool", lambda e: e.tensor_tensor(hb[:, c, s * 512:(s + 1) * 512],
                                                                hb[:, c, s * 512:(s + 1) * 512], otmp2[s][:], ALU.add),
                              reads=[b_otmp2[s], bhb[c]], writes=[bhb[c]])
                    fw.dma("sp", hA[ci * 128:(ci + 1) * 128, :], hb[:, c, :], reads=[bhb[c]], writes=[b_hA])

                nch_t = len(chunks)
                stage_a(0)
                if nch_t > 1:
                    stage_a(1)
                stage_b1(0)
                norm_tile1(ti + 1)
                for c, ci in enumerate(chunks):
                    if c + 2 < nch_t:
                        stage_a(c + 2)
                    if c + 1 < nch_t:
                        stage_b1(c + 1)
                    stage_b2(c, ci)
            fw.barrier()

    def ffn_phase(es, layer, src, b_src, supers, experts, finalize):
        moe = layer == 1
        NSC = max(len(s) for s in supers)
        d_p = fw.dsem("f%dc" % layer)
        g2 = S(es, "g2", [128, 2, D], F32); b_g2 = fw.buf("g2", d_p)
        if moe:
            fw.dma("sp", g2[:, 0, :], gb_d[5], reads=[b_gb], writes=[b_g2])
            wr = S(es, "wr", [128, 8, NE], F32); b_wr = fw.buf("wr", d_p)
            fw.dma("sp", wr[:], moe_wr.rearrange("(k p) e -> p k e", p=128), writes=[b_wr])
            fgb = S(es, "fgb", [128, D], F32); b_fgb = fw.buf("fgb", d_p)
            fw.dma("sp", fgb[:], final_g.partition_broadcast(128), writes=[b_fgb])
            x32b = [S(es, "x32_%d" % i, [128, 8, 128], F32) for i in range(2)]; b_x32b = fw.bufs("x32", 2)
            lgb = [S(es, "lg%d" % i, [128, 8, NE], F32) for i in range(2)]; b_lgb = fw.bufs("lg", 2)
            smb = [S(es, "sm%d" % i, [128, 8], F32) for i in range(2)]
        else:
            fw.dma("sp", g2[:, 0, :], gb_d[1], reads=[b_gb], writes=[b_g2])
            fw.dma("sp", g2[:, 1, :], gb_d[3], reads=[b_gb], writes=[b_g2])
        gates2 = [S(es, "gates%d" % i, [128, NSC, NE], F32) for i in range(2)]
        b_gates2 = [fw.bufs("gates%d_" % i, NSC) for i in range(2)]
        xnT2 = [S(es, "fxnT%d" % i, [128, 8, NSC * 128], BF16) for i in range(2)]
        b_xnT2 = [fw.bufs("fxnT%d_" % i, NSC) for i in range(2)]
        acc = S(es, "acc", [128, NSC, D], F32); b_acc = fw.bufs("acc", NSC)
        d_hxp = [fw.dsem("fhxp0"), fw.dsem("fhxp1")]
        hxp = [S(es, "fhxp%d" % i, [128, D], F32) for i in range(2)]
        b_hxp = [fw.buf("fhxp%d" % i, d_hxp[i]) for i in range(2)]
        d_hxf0 = fw.dsem("fhxf0")
        _hxf = S(es, "fhxf", [128, D], F32); _bhxf = fw.buf("fhxf", d_hxf0)
        hxf = [_hxf, _hxf]; b_hxf = [_bhxf, _bhxf]
        d_sl = [fw.dsem("fsl0"), fw.dsem("fsl1")]
        slg = [S(es, "slg%d" % i, [128, 8, 512], BF16) for i in range(2)]
        slu = [S(es, "slu%d" % i, [128, 8, 512], BF16) for i in range(2)]
        sld = [S(es, "sld%d" % i, [128, 4, D], BF16) for i in range(2)]
        b_sl = [fw.buf("fsl%d" % i, d_sl[i]) for i in range(2)]
        sg = [S(es, "sg%d" % i, [128, 512], F32) for i in range(2)]; b_sg = fw.bufs("sg", 2)
        hid = [S(es, "hid%d" % i, [128, 4, 512], BF16) for i in range(2)]
        b_hid = [fw.bufs("hid%d_" % i, 4) for i in range(2)]
        _ft = S(es, "ftmp", [128, D], F32); _bft = fw.buf("ftmp")
        ftmp2 = [_ft, _ft]; b_ftmp2 = [_bft, _bft]

        prep_list = [(si, c) for si in range(len(supers)) for c in range(len(supers[si]))]
        prep_loaded = [0]
        prep_done = [0]

        def prep_load(n):
            if n >= len(prep_list) or n < prep_loaded[0]:
                return
            si_, c_ = prep_list[n]
            ci_ = supers[si_][c_]
            fw.dma("sp", hxp[n % 2][:], src[ci_ * 128:(ci_ + 1) * 128, :], reads=[b_src], writes=[b_hxp[n % 2]])
            prep_loaded[0] = n + 1

        def prep_next():
            n = prep_done[0]
            if n >= len(prep_list):
                return
            prep_done[0] = n + 1
            si, c = prep_list[n]
            ci = supers[si][c]
            xb = si % 2
            xnT, b_xnT, gates, b_gates = xnT2[xb], b_xnT2[xb], gates2[xb], b_gates2[xb]
            j = modset(ci)
            prep_load(n)
            hb, bhb = hxp[n % 2], b_hxp[n % 2]
            prep_load(n + 1)
            if not moe:
                norm_transpose(hb[:], bhb, layer, 1, j, lambda k: xnT[:, k, c * 128:(c + 1) * 128], b_xnT[c], pb=(7, 7))
                return
            x32, b_x32 = x32b[n % 2], b_x32b[n % 2]
            lg, b_lg, sm = lgb[n % 2], b_lgb[n % 2], smb[n % 2]
            norm_transpose(hb[:], bhb, layer, 1, j, lambda k: x32[:, k, :], b_x32, pb=(7, 7))
            fw.op("pool", lambda e: e.tensor_copy(xnT[:, :, c * 128:(c + 1) * 128], x32[:]),
                  reads=[b_x32], writes=[b_xnT[c]])
            for k in range(8):
                fw.op("pe", lambda e: e.matmul(ps[7][:, 0:NE], lhsT=x32[:, k, :], rhs=wr[:, k, :],
                                               start=(k == 0), stop=(k == 7)),
                      reads=[b_x32, b_wr], writes=[b_ps[7]])
            L = lg[:, 0, :]
            fw.op("dve", lambda e: e.tensor_copy(L, ps[7][:, 0:NE]), reads=[b_ps[7]], writes=[b_lg])
            fw.op("dve", lambda e: e.reduce_max(sm[:, 0:1], L, axis=AX.X), reads=[b_lg], writes=[b_lg])
            fw.op("dve", lambda e: e.tensor_scalar(lg[:, 1, :], L, sm[:, 0:1], None, ALU.is_equal),
                  reads=[b_lg], writes=[b_lg])
            fw.op("dve", lambda e: e.scalar_tensor_tensor(lg[:, 2, :], lg[:, 1, :], -1e30, L, ALU.mult, ALU.add),
                  reads=[b_lg], writes=[b_lg])
            fw.op("dve", lambda e: e.reduce_max(sm[:, 1:2], lg[:, 2, :], axis=AX.X), reads=[b_lg], writes=[b_lg])
            fw.op("dve", lambda e: e.tensor_scalar(lg[:, 3, :], L, sm[:, 1:2], None, ALU.is_ge),
                  reads=[b_lg], writes=[b_lg])
            fw.op("dve", lambda e: e.tensor_scalar(sm[:, 2:3], sm[:, 0:1], -1.0, None, ALU.mult),
                  reads=[b_lg], writes=[b_lg])
            fw.op("act", lambda e: e.activation(lg[:, 4, :], L, AF.Exp, bias=sm[:, 2:3], scale=1.0),
                  reads=[b_lg], writes=[b_lg])
            fw.op("dve", lambda e: e.tensor_tensor(lg[:, 5, :], lg[:, 4, :], lg[:, 3, :], ALU.mult),
                  reads=[b_lg], writes=[b_lg])
            fw.op("dve", lambda e: e.reduce_sum(sm[:, 3:4], lg[:, 5, :], axis=AX.X), reads=[b_lg], writes=[b_lg])
            fw.op("dve", lambda e: e.reciprocal(sm[:, 4:5], sm[:, 3:4]), reads=[b_lg], writes=[b_lg])
            fw.op("dve", lambda e: e.tensor_scalar(gates[:, c, :], lg[:, 5, :], sm[:, 4:5], None, ALU.mult),
                  reads=[b_lg], writes=[b_gates[c]])

        seq = [(si, ex, hs) for si in range(len(supers)) for ex in experts for hs in range(NHS)]
        slab_loaded = [0]

        def load_slab(n):
            if n >= len(seq) or n < slab_loaded[0]:
                return
            _, ex_, hs_ = seq[n]
            si_ = n % 2
            fw.dma("sp", slg[si_][:].rearrange("p k n -> p (k n)"), wg_b[ex_, hs_], reads=[b_wf[ex_]], writes=[b_sl[si_]])
            fw.dma("sp", slu[si_][:].rearrange("p k n -> p (k n)"), wu_b[ex_, hs_], reads=[b_wf[ex_]], writes=[b_sl[si_]])
            fw.dma("sp", sld[si_][:].rearrange("p f n -> p (f n)"), wd_b[ex_, hs_], reads=[b_wf[ex_]], writes=[b_sl[si_]])
            slab_loaded[0] = n + 1

        nfin = 0
        nhid = 0
        nslab = 0
        load_slab(0)
        for _ in range(len(supers[0])):
            prep_next()
        for si, chunks in enumerate(supers):
            nsc = len(chunks)
            xb = si % 2
            xnT, b_xnT, gates, b_gates = xnT2[xb], b_xnT2[xb], gates2[xb], b_gates2[xb]
            n_next = len(supers[si + 1]) if si + 1 < len(supers) else 0
            nsteps = len(experts) * NHS
            per_step = -(-n_next // max(1, nsteps - 1))
            first = True
            ntiles = (nsc + 3) // 4
            for e_i, ex in enumerate(experts):
                for hs in range(NHS):
                    sl_i = nslab % 2
                    nslab += 1
                    load_slab(nslab)
                    pump_casts(6)
                    for t in range(ntiles):
                        tch = list(range(t * 4, min(nsc, t * 4 + 4)))
                        nt = len(tch) * 128
                        t0 = t * 512
                        hi = nhid % 2
                        nhid += 1
                        for f in range(4):
                            pg, pu = (0, 1) if f % 2 == 0 else (2, 3)
                            for k in range(8):
                                fw.op("pe", lambda e: e.matmul(ps[pg][:, 0:nt], lhsT=slg[sl_i][:, k, f * 128:(f + 1) * 128],
                                                               rhs=xnT[:, k, t0:t0 + nt], start=(k == 0), stop=(k == 7)),
                                      reads=[b_sl[sl_i]] + [b_xnT[c] for c in tch], writes=[b_ps[pg]])
                            for k in range(8):
                                fw.op("pe", lambda e: e.matmul(ps[pu][:, 0:nt], lhsT=slu[sl_i][:, k, f * 128:(f + 1) * 128],
                                                               rhs=xnT[:, k, t0:t0 + nt], start=(k == 0), stop=(k == 7)),
                                      reads=[b_sl[sl_i]] + [b_xnT[c] for c in tch], writes=[b_ps[pu]])
                            fw.op("act", lambda e: e.activation(sg[f % 2][:, 0:nt], ps[pg][:, 0:nt], AF.Silu),
                                  reads=[b_ps[pg]], writes=[b_sg[f % 2]])
                            fw.op("dve", lambda e: e.tensor_tensor(hid[hi][:, f, 0:nt], sg[f % 2][:, 0:nt], ps[pu][:, 0:nt], ALU.mult),
                                  reads=[b_sg[f % 2], b_ps[pu]], writes=[b_hid[hi][f]])
                        for cl, c in enumerate(tch):
                            for s in range(2):
                                pd = 4 + nd[0] % 3
                                nd[0] += 1
                                for f in range(4):
                                    fw.op("pe", lambda e: e.matmul(ps[pd][:], lhsT=hid[hi][:, f, cl * 128:(cl + 1) * 128],
                                                                   rhs=sld[sl_i][:, f, s * 512:(s + 1) * 512],
                                                                   start=(f == 0), stop=(f == 3)),
                                          reads=[b_hid[hi][f], b_sl[sl_i]], writes=[b_ps[pd]])
                                a_ap = acc[:, c, s * 512:(s + 1) * 512]
                                if moe:
                                    if first:
                                        fw.op("dve", lambda e: e.tensor_scalar(a_ap, ps[pd][:], gates[:, c, e_i:e_i + 1], None, ALU.mult),
                                              reads=[b_ps[pd], b_gates[c]], writes=[b_acc[c]])
                                    else:
                                        fw.op("dve", lambda e: e.scalar_tensor_tensor(a_ap, ps[pd][:], gates[:, c, e_i:e_i + 1], a_ap,
                                                                                       ALU.mult, ALU.add),
                                              reads=[b_ps[pd], b_gates[c], b_acc[c]], writes=[b_acc[c]])
                                else:
                                    if first:
                                        fw.op("dve", lambda e: e.tensor_copy(a_ap, ps[pd][:]), reads=[b_ps[pd]], writes=[b_acc[c]])
                                    else:
                                        fw.op("dve", lambda e: e.tensor_tensor(a_ap, ps[pd][:], a_ap, ALU.add),
                                              reads=[b_ps[pd], b_acc[c]], writes=[b_acc[c]])
                    first = False
                    for _ in range(per_step):
                        if prep_done[0] < sum(len(x) for x in supers[:si + 2]):
                            prep_next()
            while prep_done[0] < sum(len(x) for x in supers[:si + 2]):
                prep_next()
            def fin_load(c_, slot):
                if c_ >= len(chunks):
                    return
                ci_ = chunks[c_]
                fw.dma("sp", hxf[slot % 2][:], src[ci_ * 128:(ci_ + 1) * 128, :], reads=[b_src], writes=[b_hxf[slot % 2]])

            for c, ci in enumerate(chunks):
                j = modset(ci)
                fpar = nfin % 2
                hb, bhb = hxf[fpar], b_hxf[fpar]
                ftmp, b_ftmp = ftmp2[fpar], b_ftmp2[fpar]
                ss, b_ss = ss2[fpar], b_ss2[fpar]
                ot, bot = ftmp, b_ftmp
                fin_load(c, nfin)
                nfin += 1
                fw.op("pool", lambda e: e.tensor_tensor(ftmp[:], acc[:, c, :], g2[:, j, :], ALU.mult),
                      reads=[b_acc[c], b_g2], writes=[b_ftmp])
                if not moe:
                    fw.op("pool", lambda e: e.tensor_tensor(ot[:], ftmp[:], hb[:], ALU.add),
                          reads=[b_ftmp, bhb], writes=[bot])
                    fw.dma("sp", h1[ci * 128:(ci + 1) * 128, :], ot[:], reads=[bot], writes=[b_h1])
                else:
                    fw.op("pool", lambda e: e.tensor_tensor(ftmp[:], ftmp[:], hb[:], ALU.add),
                          reads=[b_ftmp, bhb], writes=[b_ftmp])
                    fw.op("dve", lambda e: e.memset(ss[:, 0:1], 0.0), writes=[b_ss])
                    fw.op("act", lambda e: e.activation(junk[:], ftmp[:], AF.Square, accum_out=ss[:, 0:1]),
                          reads=[b_ftmp], writes=[b_ss])
                    fw.op("act", lambda e: e.activation(ss[:, 1:2], ss[:, 0:1], AF.Sqrt, bias=eps_t[:, 0:1], scale=1.0 / D),
                          reads=[b_ss, b_eps], writes=[b_ss])
                    fw.op("dve", lambda e: e.reciprocal(ss[:, 2:3], ss[:, 1:2]), reads=[b_ss], writes=[b_ss])
                    fw.op("dve", lambda e: e.scalar_tensor_tensor(ot[:], ftmp[:], ss[:, 2:3], fgb[:], ALU.mult, ALU.mult),
                          reads=[b_ftmp, b_ss, b_fgb], writes=[bot])
                    fw.dma("sp", out_d[ci * 128:(ci + 1) * 128, :], ot[:], reads=[bot], writes=[b_out])

    nld_s = [0]
    nd = [0]

    if stop_after >= 2:
        with ExitStack() as es:
            supers = [list(range(0, 12)), list(range(12, 24)), list(range(24, 36))]
            ffn_phase(es, 0, hA, b_hA, supers, [0], None)
            fw.barrier()

    pump_casts(len(cast_jobs))
    if stop_after >= 3:
        with ExitStack() as es:
            d_p3 = fw.dsem("p3w")
            b_w3 = fw.buf("p3w", d_p3)
            b_wqs = fw.buf("wqs")
            d_cs3 = [fw.dsem("cs3_0"), fw.dsem("cs3_1")]
            cosT2 = [S(es, "cosT%d" % i, [64, 512], F32) for i in range(2)]
            sinT2 = [S(es, "sinT%d" % i, [64, 512], F32) for i in range(2)]
            b_cs32 = [fw.buf("cs3_%d" % i, d_cs3[i]) for i in range(2)]
            mk32 = S(es, "mk32", [128, 4, 128], F32); b_mk32 = fw.buf("mk32", d_p3)
            fw.dma("sp", mk32[:], masks_d, writes=[b_mk32])
            mk = S(es, "mk", [128, 4, 128], BF16); b_mk = fw.buf("mk")
            fw.op("dve", lambda e: e.tensor_copy(mk[:], mk32[:]), reads=[b_mk32], writes=[b_mk])
            snk = S(es, "snk", [128, 16], F32); b_snk = fw.buf("snk", d_p3)
            fw.dma("sp", snk[:], b_sink.partition_broadcast(128), writes=[b_snk])
            esnk = S(es, "esnk", [128, 16], F32); b_esnk = fw.buf("esnk")
            fw.op("act", lambda e: e.activation(esnk[:], snk[:], AF.Exp), reads=[b_snk], writes=[b_esnk])
            idb = S(es, "idb", [128, 128], BF16); b_idb = fw.buf("idb")
            fw.op("dve", lambda e: e.tensor_copy(idb[:], ident[:]), reads=[b_ident], writes=[b_idb])
            g1 = S(es, "g1b", [128, D], F32); b_g1 = fw.buf("g1b", d_p3)
            fw.dma("sp", g1[:], gb_d[4], reads=[b_gb], writes=[b_g1])

            kT = S(es, "kT", [64, 4, NCH * 128], BF16); b_kT = fw.bufs("kT", NCH)
            vA = S(es, "vA", [128, NCH, 4, 65], BF16); b_vA = fw.bufs("vA", NCH)
            fw.op("pool", lambda e: e.memset(vA[:, :, :, 64:65], 1.0), writes=b_vA)
            d_hx = [fw.dsem("ahx0"), fw.dsem("ahx1")]
            hx = [S(es, "ahx%d" % i, [128, 4, D], F32) for i in range(2)]
            b_hx = [[fw.buf("ahx%d_%d" % (i, c), d_hx[i]) for c in range(4)] for i in range(2)]
            xnT = S(es, "axnT", [128, 8, 512], BF16); b_xnT = fw.bufs("axnT", 4)
            r1 = S(es, "r1", [64, 512], F32); r2 = S(es, "r2", [64, 512], F32)
            b_r1 = fw.buf("r1"); b_r2 = fw.buf("r2")
            psb = ps[7][:].bitcast(BF16)

            def rope(dst_ap, b_dst, pa, pb_, slot, nt):
                fw.op("dve", lambda e: e.tensor_tensor(r1[:, 0:nt], ps[pa][0:64, 0:nt], cosT2[slot][:, 0:nt], ALU.mult),
                      reads=[b_ps[pa], b_cs32[slot]], writes=[b_r1])
                fw.op("dve", lambda e: e.tensor_tensor(r2[:, 0:nt], ps[pb_][0:64, 0:nt], sinT2[slot][:, 0:nt], ALU.mult),
                      reads=[b_ps[pb_], b_cs32[slot]], writes=[b_r2])
                fw.op("pool", lambda e: e.tensor_tensor(dst_ap, r1[:, 0:nt], r2[:, 0:nt], ALU.add),
                      reads=[b_r1, b_r2], writes=b_dst)

            esA = ExitStack()
            wk = S(esA, "wk", [128, 8, 256], BF16); wks = S(esA, "wks", [128, 8, 256], BF16)
            wv = S(esA, "wv", [128, 8, 256], BF16)
            for k in range(8):
                r = slice(k * 128, (k + 1) * 128)
                fw.dma("sp", wk[:, k, :], w_qkv_b[r, D:D + 256], reads=[b_w1], writes=[b_w3])
                fw.dma("sp", wv[:, k, :], w_qkv_b[r, D + 256:D + 512], reads=[b_w1], writes=[b_w3])

            def swap_halves(src_w, dst_w):
                sv = src_w[:].rearrange("p k (g h j) -> p (k g) h j", h=2, j=16)
                dv = dst_w[:].rearrange("p k (g h j) -> p (k g) h j", h=2, j=16)
                fw.op("pool", lambda e: e.tensor_copy(dv[:, :, 0, :], sv[:, :, 1, :]), reads=[b_w3], writes=[b_wqs])
                fw.op("pool", lambda e: e.tensor_copy(dv[:, :, 1, :], sv[:, :, 0, :]), reads=[b_w3], writes=[b_wqs])

            swap_halves(wk, wks)
            tiles = [list(range(t * 4, t * 4 + 4)) for t in range(8)] + [[32, 33], [34, 35]]
            nrot = 0
            tseq = [(ti, 0) for ti in range(10)] + [(ti, 1) for ti in range(8)]

            def load_tile3(n):
                if n >= len(tseq):
                    return
                ti_ = tseq[n][0]
                chunks_ = tiles[ti_]
                nt_ = len(chunks_) * 128
                lo_ = chunks_[0] * 128
                for c_, ci_ in enumerate(chunks_):
                    fw.dma("sp", hx[n % 2][:, c_, :], h1[ci_ * 128:(ci_ + 1) * 128, :], reads=[b_h1], writes=[b_hx[n % 2][c_]])
                fw.dma("sp", cosT2[n % 2][:, 0:nt_], cos_d[:, lo_:lo_ + nt_], writes=[b_cs32[n % 2]])
                fw.dma("sp", sinT2[n % 2][:, 0:nt_], sin_d[:, lo_:lo_ + nt_], writes=[b_cs32[n % 2]])

            xnTb = S(esA, "axnTb", [128, 8, 512], BF16); b_xnTb = fw.bufs("axnTb", 4)
            xnA = [(xnT, b_xnT), (xnTb, b_xnTb)]

            def norm_tile_a(ti_):
                if ti_ >= len(tiles):
                    return
                xn_, bxn_ = xnA[ti_ % 2]
                for c_, ci_ in enumerate(tiles[ti_]):
                    norm_transpose(hx[ti_ % 2][:, c_, :], b_hx[ti_ % 2][c_], 1, 0, modset(tiles[ti_][0]),
                                   lambda k: xn_[:, k, c_ * 128:(c_ + 1) * 128], bxn_[c_])

            load_tile3(0)
            norm_tile_a(0)
            for ti, chunks in enumerate(tiles):
                nt = len(chunks) * 128
                t_lo = ti % 2
                hb, bhb = hx[ti % 2], b_hx[ti % 2]
                j = modset(chunks[0])
                load_tile3(ti + 1)
                norm_tile_a(ti + 1)
                xnT_a, b_xnT_a = xnA[ti % 2]
                for kv in range(4):
                    pa, pb_ = (2, 3) if nrot % 2 == 0 else (4, 5)
                    nrot += 1
                    for (w_, p_) in ((wk, pa), (wks, pb_)):
                        for k in range(8):
                            fw.op("pe", lambda e: e.matmul(ps[p_][0:64, 0:nt], lhsT=w_[:, k, kv * 64:(kv + 1) * 64],
                                                           rhs=xnT_a[:, k, 0:nt], start=(k == 0), stop=(k == 7)),
                                  reads=[b_w3, b_wqs] + b_xnT_a[:len(chunks)], writes=[b_ps[p_]])
                    rope(kT[:, kv, chunks[0] * 128:chunks[0] * 128 + nt], [b_kT[ci] for ci in chunks], pa, pb_, t_lo, nt)
                for c, ci in enumerate(chunks):
                    for k in range(8):
                        fw.op("pe", lambda e: e.matmul(ps[6][:, 0:256], lhsT=xnT_a[:, k, c * 128:(c + 1) * 128], rhs=wv[:, k, :],
                                                       start=(k == 0), stop=(k == 7)),
                              reads=[b_w3, b_xnT_a[c]], writes=[b_ps[6]])
                    fw.op("act", lambda e: e.copy(vA[:, ci, :, 0:64], ps[6][:, 0:256].rearrange("p (h d) -> p h d", h=4)),
                          reads=[b_ps[6]], writes=[b_vA[ci]])
            fw.barrier()
            esA.close()
            wq = S(es, "wq", [128, 8, D], BF16); wqs = S(es, "wqs", [128, 8, D], BF16)
            wo = S(es, "wo", [128, 8, D], BF16)
            for k in range(8):
                r = slice(k * 128, (k + 1) * 128)
                fw.dma("sp", wq[:, k, :], w_qkv_b[r, 0:D], reads=[b_w1], writes=[b_w3])
                fw.dma("sp", wo[:, k, :], w_bout_b[r, :], reads=[b_w1], writes=[b_w3])
            swap_halves(wq, wqs)
            qT = S(es, "qT", [64, 16, 512], BF16); b_qT = fw.bufs("qT", 16)
            pT = [S(es, "pT%d" % i, [128, 512], BF16) for i in range(10)]; b_pT = fw.bufs("pT", 10)
            den2 = [S(es, "den%d" % i, [128, 8], F32) for i in range(2)]; b_den2 = fw.bufs("den", 2)
            otok2 = [S(es, "otok%d" % i, [128, D], BF16) for i in range(2)]; b_otok2 = fw.bufs("otok", 2)
            oT = S(es, "oT", [128, 8, 128], BF16); b_oT = fw.buf("oT")
            _ot = S(es, "aotmp", [128, 512], F32); _bot = fw.buf("aotmp")
            otmp2 = [_ot, _ot]; b_otmp2 = [_bot, _bot]
            npt_box = [0]
            for ti in range(8):
                chunks = tiles[ti]
                nt = 512
                n3 = 10 + ti
                t_lo = n3 % 2
                hb, bhb = hx[n3 % 2], b_hx[n3 % 2]
                load_tile3(n3 + 1)
                for c, ci in enumerate(chunks):
                    norm_transpose(hb[:, c, :], bhb[c], 1, 0, 0, lambda k: xnT[:, k, c * 128:(c + 1) * 128], b_xnT[c])
                for h in range(16):
                    pa, pb_ = (2, 3) if nrot % 2 == 0 else (4, 5)
                    nrot += 1
                    for (w_, p_) in ((wq, pa), (wqs, pb_)):
                        for k in range(8):
                            fw.op("pe", lambda e: e.matmul(ps[p_][0:64, 0:nt], lhsT=w_[:, k, h * 64:(h + 1) * 64],
                                                           rhs=xnT[:, k, 0:nt], start=(k == 0), stop=(k == 7)),
                                  reads=[b_w3, b_wqs] + b_xnT, writes=[b_ps[p_]])
                    rope(qT[:, h, :], [b_qT[h]], pa, pb_, t_lo, nt)
                def stage_s(c, ci, kv):
                    nonlocal_npt = npt_box
                    kprev = ci - 1 if ci > 0 else 32
                    knext = ci + 1 if ci < 31 else 33
                    mprev = 0 if ci > 0 else 2
                    mnext = 1 if ci < 31 else 3
                    klist = [(kprev, mprev), (ci, None), (knext, mnext), (34, None), (35, None)]
                    pts = []
                    for (kc, mi) in klist:
                        pi = nonlocal_npt[0] % 10
                        nonlocal_npt[0] += 1
                        bk = 2 + nonlocal_npt[0] % 3
                        fw.op("pe", lambda e: e.matmul(ps[bk][:], lhsT=kT[:, kv, kc * 128:(kc + 1) * 128],
                                                       rhs=qT[:, kv * 4:(kv + 1) * 4, c * 128:(c + 1) * 128],
                                                       start=True, stop=True),
                              reads=[b_kT[kc]] + b_qT[kv * 4:(kv + 1) * 4], writes=[b_ps[bk]])
                        fw.op("act", lambda e: e.activation(pT[pi][:], ps[bk][:], AF.Exp, scale=0.125),
                              reads=[b_ps[bk]], writes=[b_pT[pi]])
                        if mi is not None:
                            fw.op("pool", lambda e: e.tensor_tensor(
                                pT[pi][:].rearrange("p (h q) -> p h q", h=4), pT[pi][:].rearrange("p (h q) -> p h q", h=4),
                                mk[:, mi:mi + 1, :].to_broadcast([128, 4, 128]), ALU.mult),
                                reads=[b_pT[pi], b_mk], writes=[b_pT[pi]])
                        pts.append((pi, kc))
                    return pts

                def stage_pv(c, kv, pts):
                    po = 5 + kv % 2
                    ob = c % 2
                    for hh in range(4):
                        for n_, (pi, kc) in enumerate(pts):
                            fw.op("pe", lambda e: e.matmul(ps[po][:, hh * 65:(hh + 1) * 65], lhsT=pT[pi][:, hh * 128:(hh + 1) * 128],
                                                           rhs=vA[:, kc, kv, :], start=(n_ == 0), stop=(n_ == 4)),
                                  reads=[b_pT[pi], b_vA[kc]], writes=[b_ps[po]])
                    pov = ps[po][:, 0:260].rearrange("p (h d) -> p h d", h=4)
                    dn = den2[kv % 2]
                    fw.op("dve", lambda e: e.tensor_tensor(dn[:, 0:4], pov[:, :, 64], esnk[:, kv * 4:(kv + 1) * 4], ALU.add),
                          reads=[b_ps[po], b_esnk], writes=[b_den2[kv % 2]])
                    fw.op("dve", lambda e: e.reciprocal(dn[:, 4:8], dn[:, 0:4]), reads=[b_den2[kv % 2]], writes=[b_den2[kv % 2]])
                    fw.op("dve", lambda e: e.tensor_tensor(
                        otok2[ob][:, kv * 256:(kv + 1) * 256].rearrange("p (h d) -> p h d", h=4), pov[:, :, 0:64],
                        dn[:, 4:8].unsqueeze(2).to_broadcast([128, 4, 64]), ALU.mult),
                        reads=[b_ps[po], b_den2[kv % 2]], writes=[b_otok2[ob]])

                def stage_out(c, ci):
                    ob = c % 2
                    for k in range(8):
                        fw.op("pe", lambda e: e.transpose(psb[:, k * 128:(k + 1) * 128], otok2[ob][:, k * 128:(k + 1) * 128], idb[:]),
                              reads=[b_otok2[ob], b_idb], writes=[b_ps[7]])
                    fw.op("act", lambda e: e.copy(oT[:].rearrange("p k q -> p (k q)"), psb[:, :]), reads=[b_ps[7]], writes=[b_oT])
                    for s in range(2):
                        bk = s
                        for k in range(8):
                            fw.op("pe", lambda e: e.matmul(ps[bk][:], lhsT=oT[:, k, :], rhs=wo[:, k, s * 512:(s + 1) * 512],
                                                           start=(k == 0), stop=(k == 7)),
                                  reads=[b_oT, b_w3], writes=[b_ps[bk]])
                        fw.op("dve", lambda e: e.tensor_tensor(otmp2[s][:], ps[bk][:], g1[:, s * 512:(s + 1) * 512], ALU.mult),
                              reads=[b_ps[bk], b_g1], writes=[b_otmp2[s]])
                        fw.op("pool", lambda e: e.tensor_tensor(hb[:, c, s * 512:(s + 1) * 512],
                                                                hb[:, c, s * 512:(s + 1) * 512], otmp2[s][:], ALU.add),
                              reads=[b_otmp2[s], bhb[c]], writes=[bhb[c]])
                    fw.dma("sp", hB[ci * 128:(ci + 1) * 128, :], hb[:, c, :], reads=[bhb[c]], writes=[b_hB])

                items = [(c, ci, kv) for c, ci in enumerate(chunks) for kv in range(4)]
                pend = None
                for (c, ci, kv) in items:
                    pts = stage_s(c, ci, kv)
                    if pend is not None:
                        stage_pv(pend[0], pend[2], pend[3])
                        if pend[2] == 3:
                            stage_out(pend[0], pend[1])
                    pend = (c, ci, kv, pts)
                stage_pv(pend[0], pend[2], pend[3])
                stage_out(pend[0], pend[1])
            fw.barrier()


    I32 = mybir.dt.int32
    IOA = bass.IndirectOffsetOnAxis

    def moe_sparse_phase():
        NT = 23
        with ExitStack() as es:
            d_c4 = fw.dsem("p4c")
            g2r = S(es, "g2r", [128, D], F32); b_g2r = fw.buf("g2r", d_c4)
            fw.dma("sp", g2r[:], gb_d[5], reads=[b_gb], writes=[b_g2r])
            fgb = S(es, "fgb", [128, D], F32); b_fgb = fw.buf("fgb", d_c4)
            fw.dma("sp", fgb[:], final_g.partition_broadcast(128), writes=[b_fgb])
            s1i = S(es, "s1i", [128, 32], I32); s2i = S(es, "s2i", [128, 32], I32)
            w1 = S(es, "w1", [128, 32], F32); w2 = S(es, "w2", [128, 32], F32)
            widx = S(es, "widx", [128, NT * NHS], I32)
            b_idx = fw.buf("idx")

            esA = ExitStack()
            wr = S(esA, "wr", [128, 8, NE], F32); b_wr = fw.buf("wr", d_c4)
            fw.dma("sp", wr[:], moe_wr.rearrange("(k p) e -> p k e", p=128), writes=[b_wr])
            scr = S(esA, "scr", [128, D], F32); shr = S(esA, "shr", [128, D], F32); ngr = S(esA, "ngr", [128, D], F32)
            b_rows = fw.buf("rows", d_c4)
            fw.dma("sp", scr[:], gb_d[7], reads=[b_gb], writes=[b_rows])
            fw.dma("sp", shr[:], gb_d[6], reads=[b_gb], writes=[b_rows])
            fw.dma("sp", ngr[:], norm_g_raw[1, 1].partition_broadcast(128), writes=[b_rows])
            tri = S(esA, "tri", [128, 128], F32); b_tri = fw.buf("tri", d_c4)
            fw.dma("sp", tri[:], tri_d, writes=[b_tri])
            ones = S(esA, "ones", [128, 128], F32); b_ones = fw.buf("ones")
            fw.op("dve", lambda e: e.memset(ones[:], 1.0), writes=[b_ones])
            b_scr = fw.buf("scr")
            fw.op("dve", lambda e: e.scalar_tensor_tensor(scr[:], scr[:], 1.0, ngr[:], ALU.add, ALU.mult),
                  reads=[b_rows], writes=[b_scr])
            x32b = [S(esA, "x32_%d" % i, [128, 8, 128], F32) for i in range(2)]; b_x32b = fw.bufs("x32", 2)
            lgb = [S(esA, "lg%d" % i, [128, 8, NE], F32) for i in range(2)]; b_lgb = fw.bufs("lg", 2)
            smb = [S(esA, "sm%d" % i, [128, 8], F32) for i in range(2)]
            Msel = S(esA, "Msel", [128, 32, NE], F32); M1 = S(esA, "M1", [128, 32, NE], F32)
            gates = S(esA, "gates", [128, 32, NE], F32); b_M = fw.buf("M")
            xn_all = S(esA, "xn_all", [128, 32, D], BF16); b_xn = fw.bufs("xn", 32)
            xt2 = [S(esA, "xt%d" % i, [128, D], F32) for i in range(2)]; b_xt2 = fw.bufs("xt", 2)
            d_hxp = [fw.dsem("shxp0"), fw.dsem("shxp1")]
            hxp = [S(esA, "shxp%d" % i, [128, D], F32) for i in range(2)]
            b_hxp = [fw.buf("shxp%d" % i, d_hxp[i]) for i in range(2)]

            def ld(c):
                if c < 32:
                    fw.dma("sp", hxp[c % 2][:], hB[c * 128:(c + 1) * 128, :], reads=[b_hB], writes=[b_hxp[c % 2]])

            def stage_n(c):
                hb, bhb = hxp[c % 2], b_hxp[c % 2]
                ld(c + 1)
                x32, b_x32 = x32b[c % 2], b_x32b[c % 2]
                xs_, b_xs_ = norm_transpose(hb[:], bhb, 1, 1, 0, lambda k: x32[:, k, :], b_x32, pb=(7, 7))
                xt, b_xt = xt2[c % 2], b_xt2[c % 2]
                fw.op("pool", lambda e: e.tensor_tensor(xt[:], xs_[:], scr[:], ALU.mult), reads=[b_xs_, b_scr], writes=[b_xt])
                fw.op("pool", lambda e: e.tensor_tensor(xn_all[:, c, :], xt[:], shr[:], ALU.add),
                      reads=[b_xt, b_rows], writes=[b_xn[c]])

            ld(0)
            stage_n(0)
            for c in range(32):
                if c + 1 < 32:
                    stage_n(c + 1)
                x32, b_x32 = x32b[c % 2], b_x32b[c % 2]
                lg, b_lg, sm = lgb[c % 2], b_lgb[c % 2], smb[c % 2]
                for k in range(8):
                    fw.op("pe", lambda e: e.matmul(ps[6][:, 0:NE], lhsT=x32[:, k, :], rhs=wr[:, k, :],
                                                   start=(k == 0), stop=(k == 7)),
                          reads=[b_x32, b_wr], writes=[b_ps[6]])
                L = lg[:, 0, :]
                fw.op("dve", lambda e: e.tensor_copy(L, ps[6][:, 0:NE]), reads=[b_ps[6]], writes=[b_lg])
                fw.op("dve", lambda e: e.reduce_max(sm[:, 0:1], L, axis=AX.X), reads=[b_lg], writes=[b_lg])
                fw.op("dve", lambda e: e.tensor_scalar(M1[:, c, :], L, sm[:, 0:1], None, ALU.is_equal),
                      reads=[b_lg], writes=[b_M])
                fw.op("dve", lambda e: e.scalar_tensor_tensor(lg[:, 2, :], M1[:, c, :], -1e30, L, ALU.mult, ALU.add),
                      reads=[b_lg, b_M], writes=[b_lg])
                fw.op("dve", lambda e: e.reduce_max(sm[:, 1:2], lg[:, 2, :], axis=AX.X), reads=[b_lg], writes=[b_lg])
                fw.op("dve", lambda e: e.tensor_scalar(Msel[:, c, :], L, sm[:, 1:2], None, ALU.is_ge),
                      reads=[b_lg], writes=[b_M])
                fw.op("dve", lambda e: e.tensor_scalar(sm[:, 2:3], sm[:, 0:1], -1.0, None, ALU.mult),
                      reads=[b_lg], writes=[b_lg])
                fw.op("act", lambda e: e.activation(lg[:, 4, :], L, AF.Exp, bias=sm[:, 2:3], scale=1.0),
                      reads=[b_lg], writes=[b_lg])
                fw.op("dve", lambda e: e.tensor_tensor(lg[:, 5, :], lg[:, 4, :], Msel[:, c, :], ALU.mult),
                      reads=[b_lg, b_M], writes=[b_lg])
                fw.op("dve", lambda e: e.reduce_sum(sm[:, 3:4], lg[:, 5, :], axis=AX.X), reads=[b_lg], writes=[b_lg])
                fw.op("dve", lambda e: e.reciprocal(sm[:, 4:5], sm[:, 3:4]), reads=[b_lg], writes=[b_lg])
                fw.op("dve", lambda e: e.tensor_scalar(gates[:, c, :], lg[:, 5, :], sm[:, 4:5], None, ALU.mult),
                      reads=[b_lg], writes=[b_M])

            CE = 32 * NE
            Mflat = Msel[:].rearrange("p c e -> p (c e)")
            fw.op("pe", lambda e: e.matmul(ps[0][:, 0:CE], lhsT=tri[:], rhs=Mflat, start=True, stop=True),
                  reads=[b_tri, b_M], writes=[b_ps[0]])
            fw.op("pe", lambda e: e.matmul(ps[1][:, 0:CE], lhsT=ones[:], rhs=Mflat, start=True, stop=True),
                  reads=[b_ones, b_M], writes=[b_ps[1]])
            P1 = S(esA, "P1", [128, 32, NE], F32); T = S(esA, "T", [128, 32, NE], F32)
            Cpre = S(esA, "Cpre", [128, 32, NE], F32); slot = S(esA, "slot", [128, 32, NE], F32)
            M2 = S(esA, "M2", [128, 32, NE], F32); tmp3 = S(esA, "tmp3", [128, 32, NE], F32)
            sml = S(esA, "sml", [128, 64], F32)
            s1f = S(esA, "s1f", [128, 32], F32); s2f = S(esA, "s2f", [128, 32], F32)
            tvi = S(esA, "tvi", [128, NT], I32); tv = S(esA, "tv", [128, NT], F32); et = S(esA, "et", [128, NT], F32)
            csti = S(esA, "csti", [128, NHS], I32); cst = S(esA, "cst", [128, NHS], F32)
            widxf = S(esA, "widxf", [128, NT, NHS], F32)
            b_B = fw.buf("B")
            V = lambda fn, r=(), w=(): fw.op("dve", fn, reads=list(r) + [b_B], writes=list(w) + [b_B])
            V(lambda e: e.tensor_copy(P1[:].rearrange("p c e -> p (c e)"), ps[0][:, 0:CE]), r=[b_ps[0]])
            V(lambda e: e.tensor_copy(T[:].rearrange("p c e -> p (c e)"), ps[1][:, 0:CE]), r=[b_ps[1]])
            V(lambda e: e.memset(Cpre[:, 0, :], 0.0))
            for c in range(1, 32):
                V(lambda e: e.tensor_tensor(Cpre[:, c, :], Cpre[:, c - 1, :], T[:, c - 1, :], ALU.add))
            cnt, nt8, tb, base = sml[:, 0:8], sml[:, 8:16], sml[:, 16:25], sml[:, 32:40]
            V(lambda e: e.tensor_tensor(cnt, Cpre[:, 31, :], T[:, 31, :], ALU.add))
            V(lambda e: e.tensor_scalar(nt8, cnt, 0.0, None, ALU.is_gt))
            for jj in range(1, 8):
                V(lambda e: e.scalar_tensor_tensor(nt8, cnt, 512.0 * jj, nt8, ALU.is_gt, ALU.add))
            V(lambda e: e.memset(sml[:, 16:17], 0.0))
            for ee in range(NE):
                V(lambda e: e.tensor_tensor(sml[:, 17 + ee:18 + ee], sml[:, 16 + ee:17 + ee], sml[:, 8 + ee:9 + ee], ALU.add))
            V(lambda e: e.tensor_scalar(base, sml[:, 16:24], 512.0, None, ALU.mult))
            V(lambda e: e.tensor_tensor(slot[:], P1[:], Cpre[:], ALU.add))
            V(lambda e: e.tensor_tensor(slot[:], slot[:], base.unsqueeze(1).to_broadcast([128, 32, NE]), ALU.add))
            V(lambda e: e.tensor_tensor(M2[:], Msel[:], M1[:], ALU.subtract), r=[b_M])
            V(lambda e: e.tensor_tensor(tmp3[:], M1[:], slot[:], ALU.mult), r=[b_M])
            V(lambda e: e.reduce_sum(s1f[:], tmp3[:], axis=AX.X))
            V(lambda e: e.tensor_tensor(tmp3[:], M2[:], slot[:], ALU.mult))
            V(lambda e: e.reduce_sum(s2f[:], tmp3[:], axis=AX.X))
            V(lambda e: e.tensor_tensor(tmp3[:], M1[:], gates[:], ALU.mult), r=[b_M])
            V(lambda e: e.reduce_sum(w1[:], tmp3[:], axis=AX.X), w=[b_idx])
            V(lambda e: e.tensor_tensor(tmp3[:], M2[:], gates[:], ALU.mult), r=[b_M])
            V(lambda e: e.reduce_sum(w2[:], tmp3[:], axis=AX.X), w=[b_idx])
            V(lambda e: e.tensor_copy(s1i[:], s1f[:]), w=[b_idx])
            V(lambda e: e.tensor_copy(s2i[:], s2f[:]), w=[b_idx])
            b_io = fw.buf("io")
            fw.op("pool", lambda e: e.iota(tvi[:], [[1, NT]], base=0, channel_multiplier=0), writes=[b_io])
            fw.op("pool", lambda e: e.iota(csti[:], [[128, NHS]], base=0, channel_multiplier=1), writes=[b_io])
            V(lambda e: e.tensor_copy(tv[:], tvi[:]), r=[b_io])
            V(lambda e: e.tensor_copy(cst[:], csti[:]), r=[b_io])
            V(lambda e: e.memset(et[:], 0.0))
            for ee in range(NE - 1):
                V(lambda e: e.scalar_tensor_tensor(et[:], tv[:], sml[:, 17 + ee:18 + ee], et[:], ALU.is_ge, ALU.add))
            V(lambda e: e.tensor_scalar(et[:], et[:], float(NHS * 128), None, ALU.mult))
            V(lambda e: e.tensor_tensor(widxf[:], et[:].unsqueeze(2).to_broadcast([128, NT, NHS]),
                                        cst[:].unsqueeze(1).to_broadcast([128, NT, NHS]), ALU.add))
            V(lambda e: e.tensor_copy(widx[:].rearrange("p (t h) -> p t h", h=NHS), widxf[:]), w=[b_idx])

            for c in range(32):
                fw.idma(XS[:, :], IOA(ap=s1i[:, c:c + 1], axis=0), xn_all[:, c, :], None,
                        reads=[b_xn[c], b_idx], writes=[b_XS], dsem=d_XS)
                fw.idma(XS[:, :], IOA(ap=s2i[:, c:c + 1], axis=0), xn_all[:, c, :], None,
                        reads=[b_xn[c], b_idx], writes=[b_XS], dsem=d_XS)
            fw.barrier()
            esA.close()

            esD = ExitStack()
            NSB = 3
            d_sl = [fw.dsem("ssl%d" % i) for i in range(NSB)]
            slab = [S(esD, "slab%d" % i, [128, 3 * 4096], BF16) for i in range(NSB)]
            b_sl = [fw.buf("ssl%d" % i, d_sl[i]) for i in range(NSB)]
            slg = [slab[i][:, 0:4096].rearrange("p (k n) -> p k n", k=8) for i in range(NSB)]
            slu = [slab[i][:, 4096:8192].rearrange("p (k n) -> p k n", k=8) for i in range(NSB)]
            sld = [slab[i][:, 8192:12288].rearrange("p (f n) -> p f n", f=4) for i in range(NSB)]
            sg = [S(esD, "ssg%d" % i, [128, 512], F32) for i in range(2)]; b_sg = fw.bufs("ssg", 2)
            hid = [S(esD, "shid%d" % i, [128, 4, 512], BF16) for i in range(2)]
            b_hid = [fw.bufs("shid%d_" % i, 4) for i in range(2)]
            d_xsl = [fw.dsem("xsl0"), fw.dsem("xsl1")]
            xsl = [S(esD, "xsl%d" % i, [128, 4, D], BF16) for i in range(2)]
            b_xsl = [fw.buf("xsl%d" % i, d_xsl[i]) for i in range(2)]
            xsT = [S(esD, "xsT%d" % i, [128, 8, 512], BF16) for i in range(2)]
            b_xsT = [fw.bufs("xsT%d_" % i, 4) for i in range(2)]
            accY = [S(esD, "accY%d" % i, [128, 4, D], F32) for i in range(2)]
            b_accY = [fw.bufs("accY%d_" % i, 4) for i in range(2)]
            idb4 = S(esD, "idb4", [128, 128], BF16); b_idb4 = fw.buf("idb4")
            fw.op("dve", lambda e: e.tensor_copy(idb4[:], ident[:]), reads=[b_ident], writes=[b_idb4])
            psb7 = ps[7][:].bitcast(BF16)
            b_wall = [b_wf[1 + e_] for e_ in range(NE)]

            def load_tile(t):
                if t < NT:
                    fw.dma("sp", xsl[t % 2][:], XS[t * 512:(t + 1) * 512, :].rearrange("(c p) d -> p c d", p=128),
                           reads=[b_XS], writes=[b_xsl[t % 2]])

            def prep_tile(t):
                if t >= NT:
                    return
                for c in range(4):
                    for k in range(8):
                        fw.op("pe", lambda e: e.transpose(psb7[:, k * 128:(k + 1) * 128], xsl[t % 2][:, c, k * 128:(k + 1) * 128], idb4[:]),
                              reads=[b_xsl[t % 2], b_idb4], writes=[b_ps[7]])
                    fw.op("act", lambda e: e.copy(xsT[t % 2][:, :, c * 128:(c + 1) * 128],
                                                  psb7[:, :].rearrange("p (k q) -> p k q", k=8)),
                          reads=[b_ps[7]], writes=[b_xsT[t % 2][c]])

            def load_slab(n):
                if n < NT * NHS:
                    fw.idma(slab[n % NSB][:, :], None, wgud[:, :], IOA(ap=widx[:, n:n + 1], axis=0),
                            reads=[b_idx] + b_wall, writes=[b_sl[n % NSB]], dsem=d_sl[n % NSB])

            load_tile(0)
            load_tile(1)
            prep_tile(0)
            load_slab(0)
            load_slab(1)
            ndd = 0
            nhid = 0
            for t in range(NT):
                xT, b_xT = xsT[t % 2], b_xsT[t % 2]
                aY, b_aY = accY[t % 2], b_accY[t % 2]
                for hs in range(NHS):
                    n = t * NHS + hs
                    si = n % NSB
                    load_slab(n + 2)
                    hi = nhid % 2
                    nhid += 1
                    for f in range(4):
                        pg, pu = (0, 1) if f % 2 == 0 else (2, 3)
                        for k in range(8):
                            fw.op("pe", lambda e: e.matmul(ps[pg][:], lhsT=slg[si][:, k, f * 128:(f + 1) * 128],
                                                           rhs=xT[:, k, :], start=(k == 0), stop=(k == 7)),
                                  reads=[b_sl[si]] + b_xT, writes=[b_ps[pg]])
                        for k in range(8):
                            fw.op("pe", lambda e: e.matmul(ps[pu][:], lhsT=slu[si][:, k, f * 128:(f + 1) * 128],
                                                           rhs=xT[:, k, :], start=(k == 0), stop=(k == 7)),
                                  reads=[b_sl[si]] + b_xT, writes=[b_ps[pu]])
                        fw.op("act", lambda e: e.activation(sg[f % 2][:], ps[pg][:], AF.Silu),
                              reads=[b_ps[pg]], writes=[b_sg[f % 2]])
                        fw.op("dve", lambda e: e.tensor_tensor(hid[hi][:, f, :], sg[f % 2][:], ps[pu][:], ALU.mult),
                              reads=[b_sg[f % 2], b_ps[pu]], writes=[b_hid[hi][f]])
                    for c in range(4):
                        for s2_ in range(2):
                            pd = 4 + ndd % 3
                            ndd += 1
                            for f in range(4):
                                fw.op("pe", lambda e: e.matmul(ps[pd][:], lhsT=hid[hi][:, f, c * 128:(c + 1) * 128],
                                                               rhs=sld[si][:, f, s2_ * 512:(s2_ + 1) * 512],
                                                               start=(f == 0), stop=(f == 3)),
                                      reads=[b_hid[hi][f], b_sl[si]], writes=[b_ps[pd]])
                            a_ap = aY[:, c, s2_ * 512:(s2_ + 1) * 512]
                            if hs == 0:
                                fw.op("dve", lambda e: e.tensor_copy(a_ap, ps[pd][:]), reads=[b_ps[pd]], writes=[b_aY[c]])
                            else:
                                fw.op("dve", lambda e: e.tensor_tensor(a_ap, ps[pd][:], a_ap, ALU.add),
                                      reads=[b_ps[pd], b_aY[c]], writes=[b_aY[c]])
                    if hs == 3:
                        prep_tile(t + 1)
                        load_tile(t + 2)
                for c in range(4):
                    fw.dma("sp", YS[t * 512 + c * 128:t * 512 + (c + 1) * 128, :], aY[:, c, :],
                           reads=[b_aY[c]], writes=[b_YS])
            fw.barrier()
            esD.close()

            d_y = [fw.dsem("ya0"), fw.dsem("ya1")]
            ya = [S(es, "ya%d" % i, [128, D], F32) for i in range(2)]
            yb = [S(es, "yb%d" % i, [128, D], F32) for i in range(2)]
            b_y = [fw.buf("y%d" % i, d_y[i]) for i in range(2)]
            d_hf = [fw.dsem("hf0"), fw.dsem("hf1")]
            hf_ = [S(es, "hf%d" % i, [128, D], F32) for i in range(2)]
            b_hf = [fw.buf("hf%d" % i, d_hf[i]) for i in range(2)]
            ft2 = [S(es, "ft%d" % i, [128, D], F32) for i in range(2)]; b_ft2 = fw.bufs("ft", 2)

            def load_fin(c):
                if c >= 32:
                    return
                sl_ = c % 2
                fw.idma(ya[sl_][:, :], None, YS[:, :], IOA(ap=s1i[:, c:c + 1], axis=0), reads=[b_YS, b_idx], writes=[b_y[sl_]])
                fw.idma(yb[sl_][:, :], None, YS[:, :], IOA(ap=s2i[:, c:c + 1], axis=0), reads=[b_YS, b_idx], writes=[b_y[sl_]])
                fw.dma("sp", hf_[sl_][:], hB[c * 128:(c + 1) * 128, :], reads=[b_hB], writes=[b_hf[sl_]])

            load_fin(0)
            for c in range(32):
                sl_ = c % 2
                load_fin(c + 1)
                ft, b_ft = ft2[sl_], b_ft2[sl_]
                ss, b_ss = ss2[sl_], b_ss2[sl_]
                fw.op("dve", lambda e: e.tensor_scalar(ft[:], ya[sl_][:], w1[:, c:c + 1], None, ALU.mult),
                      reads=[b_y[sl_], b_idx], writes=[b_ft])
                fw.op("dve", lambda e: e.scalar_tensor_tensor(ft[:], yb[sl_][:], w2[:, c:c + 1], ft[:], ALU.mult, ALU.add),
                      reads=[b_y[sl_], b_idx, b_ft], writes=[b_ft])
                fw.op("dve", lambda e: e.tensor_tensor(ft[:], ft[:], g2r[:], ALU.mult), reads=[b_ft, b_g2r], writes=[b_ft])
                fw.op("dve", lambda e: e.tensor_tensor(ft[:], ft[:], hf_[sl_][:], ALU.add), reads=[b_ft, b_hf[sl_]], writes=[b_ft])
                fw.op("dve", lambda e: e.memset(ss[:, 0:1], 0.0), writes=[b_ss])
                fw.op("act", lambda e: e.activation(junk[:], ft[:], AF.Square, accum_out=ss[:, 0:1]),
                      reads=[b_ft], writes=[b_ss])
                fw.op("act", lambda e: e.activation(ss[:, 1:2], ss[:, 0:1], AF.Sqrt, bias=eps_t[:, 0:1], scale=1.0 / D),
                      reads=[b_ss, b_eps], writes=[b_ss])
                fw.op("dve", lambda e: e.reciprocal(ss[:, 2:3], ss[:, 1:2]), reads=[b_ss], writes=[b_ss])
                fw.op("dve", lambda e: e.scalar_tensor_tensor(ft[:], ft[:], ss[:, 2:3], fgb[:], ALU.mult, ALU.mult),
                      reads=[b_ft, b_ss, b_fgb], writes=[b_ft])
                fw.dma("sp", out_d[c * 128:(c + 1) * 128, :], ft[:], reads=[b_ft], writes=[b_out])
            fw.barrier()

    if stop_after >= 4:
        moe_sparse_phase()

    fw.finish("sp")
    return nc


def _rope_tables(hf):
    pos = np.concatenate([
        hf * OWN + np.arange(OWN),
        hf * OWN - 128 + np.arange(128),
        hf * OWN + OWN + np.arange(128),
    ])
    valid = (pos >= 0) & (pos < SEQ)
    pos = np.clip(pos, 0, SEQ - 1)
    row = (pos // 64).astype(np.float32)
    col = (pos % 64).astype(np.float32)
    inv = (np.float32(10000.0) ** (-np.arange(16, dtype=np.float32) / np.float32(16))).astype(np.float32)
    cos_t = np.ones((64, NCH * 128), np.float32)
    sin_t = np.zeros((64, NCH * 128), np.float32)
    n = pos.shape[0]
    for d in range(64):
        p = row if d < 32 else col
        jj = d % 16
        sign = -1.0 if (d % 32) < 16 else 1.0
        ang = (p * inv[jj]).astype(np.float32)
        cos_t[d, :n] = np.cos(ang).astype(np.float32)
        sin_t[d, :n] = (sign * np.sin(ang)).astype(np.float32)
    del valid
    return cos_t, sin_t


def _masks(hf):
    kk = np.arange(128)[:, None]
    q = np.arange(128)[None, :]
    prev = (kk >= q).astype(np.float32)
    nxt = (kk <= q).astype(np.float32)
    m = np.zeros((128, 4, 128), np.float32)
    m[:, 0] = prev
    m[:, 1] = nxt
    m[:, 2] = prev if hf == 1 else 0.0
    m[:, 3] = nxt if hf == 0 else 0.0
    return m


def make_in_maps(inp):
    f = lambda a: np.ascontiguousarray(np.asarray(a, dtype=np.float32))
    x = f(inp["x"]); c = f(inp["c"]); ctx = f(inp["ctx"]); c_ctx = f(inp["c_ctx"])
    shared = {
        "ada_w": f(inp["ada_w"]),
        "ada_bT": f(np.asarray(inp["ada_b"]).reshape(2, 48, 128).transpose(2, 0, 1)),
        "ada_b": f(inp["ada_b"]),
        "norm_gT": f(np.asarray(inp["norm_g"]).reshape(2, 2, 8, 128).transpose(3, 0, 1, 2).reshape(128, 32)),
        "final_g": f(inp["final_g"]),
        "a_w_in": f(inp["a_w_in"][0]), "a_v_g": f(inp["a_v_g"][0]), "a_v_b": f(inp["a_v_b"][0]),
        "a_w_s": f(inp["a_w_s"][0]), "a_b_sT": f(np.asarray(inp["a_b_s"][0]).T),
        "a_w_out": f(inp["a_w_out"][0]),
        "b_w_qkv": f(inp["b_w_qkv"][0]), "b_sink": f(inp["b_sink"][0]), "b_w_out": f(inp["b_w_out"][0]),
        "ffn_w_gate": f(inp["ffn_w_gate"][0]), "ffn_w_up": f(inp["ffn_w_up"][0]), "ffn_w_down": f(inp["ffn_w_down"][0]),
        "moe_w_router": f(inp["moe_w_router"][0]),
        "moe_w_gate": f(inp["moe_w_gate"][0]), "moe_w_up": f(inp["moe_w_up"][0]), "moe_w_down": f(inp["moe_w_down"][0]),
        "ident": np.eye(128, dtype=np.float32),
        "tri": np.triu(np.ones((128, 128), np.float32), 1),
        "norm_g_raw": f(inp["norm_g"]),
    }
    maps = []
    for r in range(8):
        b, hf = r // 2, r % 2
        x_loc = np.zeros((OWN + 256, D), np.float32)
        x_loc[:OWN] = x[b, hf * OWN:(hf + 1) * OWN]
        if hf == 1:
            x_loc[OWN:OWN + 128] = x[b, OWN - 128:OWN]
        else:
            x_loc[OWN + 128:OWN + 256] = x[b, OWN:OWN + 128]
        c2 = np.stack([c[b], c_ctx], axis=-1).reshape(8, 128, 2).transpose(1, 0, 2).reshape(128, 16)
        cos_t, sin_t = _rope_tables(hf)
        m = dict(shared)
        m.update({"x_loc": x_loc, "ctx_b": f(ctx[b]), "c2": f(c2), "cos_t": cos_t, "sin_t": sin_t,
                  "masks": _masks(hf)})
        maps.append(m)
    return maps


_NC_CACHE = {}


def kernel(**inputs):
    if "nc" not in _NC_CACHE:
        _NC_CACHE["nc"] = build()
    nc = _NC_CACHE["nc"]
    maps = make_in_maps(inputs)
    res = run_bass_kernel_spmd(nc, maps, core_ids=list(range(8)))
    out = np.zeros((4, SEQ, D), np.float32)
    for r in range(8):
        b, hf = r // 2, r % 2
        out[b, hf * OWN:(hf + 1) * OWN] = res.results[r]["out"]
    return out
```

```python
from contextlib import ExitStack
import numpy as np
import concourse.bass as bass
import concourse.mybir as mybir
from concourse.bass_utils import run_bass_kernel_spmd

F32 = mybir.dt.float32
BF16 = mybir.dt.bfloat16
AF = mybir.ActivationFunctionType
ALU = mybir.AluOpType
AX = mybir.AxisListType

D = 1024
DFF = 3584
NHS = 7
NE = 8
SEQ = 8192
OWN = 4096
NCH = 36
EPS = 1e-6


class DSem:
    def __init__(self, fw, name):
        self.sem = fw.new_sem(name)
        self.count = 0
        self.key = "d:%s:%d" % (name, fw._nsem)


class Buf:
    def __init__(self, name, dsem=None):
        self.name = name
        self.dsem = dsem
        self.last_w = None
        self.readers = {}


class FW:
    def __init__(self, nc):
        self.nc = nc
        self._stack = []
        self.eng = {"pe": nc.tensor, "act": nc.scalar, "dve": nc.vector,
                    "pool": nc.gpsimd, "sp": nc.sync}
        self.sem = {}
        self.cnt = {}
        for k in ("pe", "act", "dve", "pool"):
            self.sem[k] = self.new_sem("e_" + k)
            self.cnt[k] = 0
        self.waited = {k: {} for k in self.eng}
        self.dsems = []

    def new_sem(self, name):
        self._nsem = getattr(self, "_nsem", 0) + 1
        cm = self.nc.semaphore("%s_%d" % (name, self._nsem))
        s = cm.__enter__()
        self._stack.append(cm)
        return s

    def dsem(self, name, async_=False):
        d = DSem(self, name)
        d.async_ = async_
        self.dsems.append(d)
        return d

    def buf(self, name, dsem=None):
        return Buf(name, dsem)

    def bufs(self, name, n, dsem=None):
        return [Buf("%s%d" % (name, i), dsem) for i in range(n)]

    def _wait(self, eng, tok):
        if tok is None:
            return
        sem, val, key = tok[:3]
        if len(tok) > 3:
            val = tok[3].count
            tok[3].waited_since_issue = True
        if self.waited[eng].get(key, 0) >= val:
            return
        self.eng[eng].wait_ge(sem, val)
        self.waited[eng][key] = val

    def _deps(self, eng, reads, writes):
        for b in reads:
            t = b.last_w
            if t is not None:
                if t[2] == eng and eng == "pe":
                    continue
                self._wait(eng, t)
        for b in writes:
            t = b.last_w
            if t is not None and t[2] != eng:
                self._wait(eng, t)
            for k, t in b.readers.items():
                if k != eng:
                    self._wait(eng, t)

    def _commit(self, tok, reads, writes):
        for b in reads:
            b.readers[tok[2]] = tok
        for b in writes:
            b.last_w = tok
            b.readers = {}

    def op(self, eng, fn, reads=(), writes=()):
        self._deps(eng, reads, writes)
        ins = fn(self.eng[eng])
        self.cnt[eng] += 1
        ins.then_inc(self.sem[eng], 1)
        tok = (self.sem[eng], self.cnt[eng], eng)
        self._commit(tok, reads, writes)
        return ins

    def dma(self, q, out, in_, reads=(), writes=(), dsem=None, **kw):
        if dsem is None:
            dsem = writes[0].dsem
        assert dsem is not None
        self._deps(q, reads, writes)
        if getattr(dsem, "waited_since_issue", False) and dsem.count > 0:
            self._wait(q, (dsem.sem, dsem.count, dsem.key))
        dsem.waited_since_issue = False
        ins = self.eng[q].dma_start(out=out, in_=in_, **kw)
        dsem.count += 16
        ins.then_inc(dsem.sem, 16)
        tok = (dsem.sem, dsem.count, dsem.key, dsem)
        self._commit(tok, reads, writes)
        return ins

    def idma(self, out, out_off, in_, in_off, reads=(), writes=(), dsem=None):
        q = "pool"
        if dsem is None:
            dsem = writes[0].dsem
        self._deps(q, reads, writes)
        if getattr(dsem, "waited_since_issue", False) and dsem.count > 0:
            self._wait(q, (dsem.sem, dsem.count, dsem.key))
        dsem.waited_since_issue = False
        ins = self.nc.gpsimd.indirect_dma_start(out=out, out_offset=out_off, in_=in_, in_offset=in_off)
        dsem.count += 16
        ins.then_inc(dsem.sem, 16)
        tok = (dsem.sem, dsem.count, dsem.key, dsem)
        self._commit(tok, reads, writes)
        return ins

    def barrier(self):
        toks = [(self.sem[k], self.cnt[k], k) for k in self.sem if self.cnt[k] > 0]
        toks += [(d.sem, d.count, d.key) for d in self.dsems if d.count > 0 and not d.async_]
        for e in self.eng:
            for t in toks:
                if t[2] != e:
                    self._wait(e, t)

    def finish(self, eng="sp"):
        toks = [(d.sem, d.count, d.key) for d in self.dsems if d.count > 0]
        toks += [(self.sem[k], self.cnt[k], k) for k in self.sem if self.cnt[k] > 0]
        for t in toks:
            self._wait(eng, t)


def build(stop_after=99, debug=False):
    nc = bass.Bass("TRN2", target_bir_lowering=False)
    fw = FW(nc)

    def din(name, shape, dt=F32):
        return nc.dram_tensor(name, list(shape), dt, kind="ExternalInput").ap()

    def dscr(name, shape, dt=F32):
        kind = "ExternalOutput" if (debug and dt == F32) else "Internal"
        return nc.dram_tensor(name, list(shape), dt, kind=kind).ap()

    x_loc = din("x_loc", [OWN + 256, D])
    ctx_b = din("ctx_b", [256, D])
    c2 = din("c2", [128, 16])
    ada_w = din("ada_w", [2, D, 6 * D])
    ada_bT = din("ada_bT", [128, 2, 48])
    ada_b = din("ada_b", [2, 6 * D])
    norm_gT = din("norm_gT", [128, 32])
    norm_g_raw = din("norm_g_raw", [2, 2, D])
    tri_d = din("tri", [128, 128])
    final_g = din("final_g", [D])
    a_w_in = din("a_w_in", [D, 2 * D])
    a_v_g = din("a_v_g", [D])
    a_v_b = din("a_v_b", [D])
    a_w_s = din("a_w_s", [16, 128, 128])
    a_b_sT = din("a_b_sT", [128, 16])
    a_w_out = din("a_w_out", [D, D])
    b_w_qkv = din("b_w_qkv", [D, 1536])
    b_sink = din("b_sink", [16])
    b_w_out = din("b_w_out", [D, D])
    ffn_wg = din("ffn_w_gate", [D, DFF])
    ffn_wu = din("ffn_w_up", [D, DFF])
    ffn_wd = din("ffn_w_down", [DFF, D])
    if stop_after >= 4:
        moe_wr = din("moe_w_router", [D, NE])
        moe_wg = din("moe_w_gate", [NE, D, DFF])
        moe_wu = din("moe_w_up", [NE, D, DFF])
        moe_wd = din("moe_w_down", [NE, DFF, D])
    ident_d = din("ident", [128, 128])
    cos_d = din("cos_t", [64, NCH * 128])
    sin_d = din("sin_t", [64, NCH * 128])
    masks_d = din("masks", [128, 4, 128])

    out_d = nc.dram_tensor("out", [OWN, D], F32, kind="ExternalOutput").ap()

    hA = dscr("hA", [NCH * 128, D])
    h1 = dscr("h1", [NCH * 128, D])
    hB = dscr("hB", [OWN, D])
    gb_d = dscr("gb_d", [8, 128, D])
    w_in_b = dscr("w_in_b", [D, 2 * D], BF16)
    w_aout_b = dscr("w_aout_b", [D, D], BF16)
    w_qkv_b = dscr("w_qkv_b", [D, 1536], BF16)
    w_bout_b = dscr("w_bout_b", [D, D], BF16)
    wg_b = dscr("wg_b", [1, NHS, 128, 8 * 512], BF16)
    wu_b = dscr("wu_b", [1, NHS, 128, 8 * 512], BF16)
    wd_b = dscr("wd_b", [1, NHS, 128, 4 * D], BF16)
    wgud = dscr("wgud", [NE * NHS * 128, 3 * 4096], BF16)
    NSLOT = 24 * 512
    XS = dscr("XS", [NSLOT, D], BF16)
    YS = nc.dram_tensor("YS", [NSLOT, D], F32, kind="Internal").ap()
    d_XS = fw.dsem("XS"); b_XS = fw.buf("XS", d_XS)
    d_YS = fw.dsem("YS"); b_YS = fw.buf("YS", d_YS)

    d_out = fw.dsem("out")
    b_out = fw.buf("out", d_out)
    d_hA = fw.dsem("hA"); b_hA = fw.buf("hA", d_hA)
    d_h1 = fw.dsem("h1"); b_h1 = fw.buf("h1", d_h1)
    d_hB = fw.dsem("hB"); b_hB = fw.buf("hB", d_hB)
    d_gb = fw.dsem("gb"); b_gb = fw.buf("gb", d_gb)
    d_w0 = fw.dsem("w0", True); b_w0 = fw.buf("w0", d_w0)
    d_w1 = fw.dsem("w1", True); b_w1 = fw.buf("w1", d_w1)
    d_wf = [fw.dsem("wf%d" % e, True) for e in range(1 + NE)]
    b_wf = [fw.buf("wf%d" % e, d_wf[e]) for e in range(1 + NE)]

    cast_jobs = []

    def cast(dst, src, b):
        cast_jobs.append((dst, src, b))

    def pump_casts(n):
        for _ in range(n):
            if cast_jobs:
                dst, src, b = cast_jobs.pop(0)
                fw.dma("pool", dst, src, writes=[b])

    for j in range(4):
        cast(w_in_b[j * 256:(j + 1) * 256, :], a_w_in[j * 256:(j + 1) * 256, :], b_w0)
    for j in range(2):
        cast(w_aout_b[j * 512:(j + 1) * 512, :], a_w_out[j * 512:(j + 1) * 512, :], b_w0)

    def cast_ffn(e, wg, wu, wd):
        for hs in range(NHS):
            cast(wg_b[e, hs].rearrange("p (k n) -> p k n", k=8),
                 wg[:, hs * 512:(hs + 1) * 512].rearrange("(k p) n -> p k n", p=128), b_wf[e])
            cast(wu_b[e, hs].rearrange("p (k n) -> p k n", k=8),
                 wu[:, hs * 512:(hs + 1) * 512].rearrange("(k p) n -> p k n", p=128), b_wf[e])
            cast(wd_b[e, hs].rearrange("p (f n) -> p f n", f=4),
                 wd[hs * 512:(hs + 1) * 512, :].rearrange("(f p) n -> p f n", p=128), b_wf[e])

    pump_casts(len(cast_jobs))
    if stop_after >= 2:
        cast_ffn(0, ffn_wg, ffn_wu, ffn_wd)
    if stop_after >= 3:
        for j in range(4):
            cast(w_qkv_b[j * 256:(j + 1) * 256, :], b_w_qkv[j * 256:(j + 1) * 256, :], b_w1)
        for j in range(2):
            cast(w_bout_b[j * 512:(j + 1) * 512, :], b_w_out[j * 512:(j + 1) * 512, :], b_w1)
    if stop_after >= 4:
        for e in range(NE):
            for hs in range(NHS):
                rows = wgud[(e * NHS + hs) * 128:(e * NHS + hs + 1) * 128, :]
                cast(rows[:, 0:4096].rearrange("p (k n) -> p k n", k=8),
                     moe_wg[e][:, hs * 512:(hs + 1) * 512].rearrange("(k p) n -> p k n", p=128), b_wf[1 + e])
                cast(rows[:, 4096:8192].rearrange("p (k n) -> p k n", k=8),
                     moe_wu[e][:, hs * 512:(hs + 1) * 512].rearrange("(k p) n -> p k n", p=128), b_wf[1 + e])
                cast(rows[:, 8192:12288].rearrange("p (f n) -> p f n", f=4),
                     moe_wd[e][hs * 512:(hs + 1) * 512, :].rearrange("(f p) n -> p f n", p=128), b_wf[1 + e])

    gst = ExitStack()
    ps = [gst.enter_context(nc.psum_tensor("ps%d" % i, [128, 512], F32)) for i in range(8)]
    b_ps = fw.bufs("ps", 8)

    nuid = [0]

    def S(es, name, shape, dt):
        nuid[0] += 1
        return es.enter_context(nc.sbuf_tensor("s%d_%s" % (nuid[0], name), list(shape), dt))

    d_c = fw.dsem("const")
    ident = S(gst, "ident", [128, 128], F32); b_ident = fw.buf("ident", d_c)
    fw.dma("sp", ident[:], ident_d, writes=[b_ident])
    modT = S(gst, "modT", [128, 2, 48, 2], F32); b_modT = fw.buf("modT")
    scl = S(gst, "scl", [128, 2, 2, 2, 8], F32); b_scl = fw.buf("scl")
    eps_t = S(gst, "eps_t", [128, 1], F32); b_eps = fw.buf("eps")
    fw.op("dve", lambda e: e.memset(eps_t[:], EPS), writes=[b_eps])
    ss2 = [S(gst, "ss%d" % i, [128, 4], F32) for i in range(2)]; b_ss2 = fw.bufs("ss", 2)
    junk = S(gst, "junk", [128, D], BF16); b_junk = fw.buf("junk")
    xs2 = [S(gst, "xs%d" % i, [128, D], F32) for i in range(2)]; b_xs2 = fw.bufs("xs", 2)
    ss, b_ss = ss2[0], b_ss2[0]
    nnt = [0]

    with ExitStack() as es:
        cs = S(es, "cs", [128, 8, 2], F32); b_cs = fw.buf("cs", d_c)
        fw.dma("sp", cs[:].rearrange("p k j -> p (k j)"), c2, writes=[b_cs])
        csil = S(es, "csil", [128, 8, 2], F32); b_csil = fw.buf("csil")
        fw.op("act", lambda e: e.activation(csil[:], cs[:], AF.Silu), reads=[b_cs], writes=[b_csil])
        crep = S(es, "crep", [128, 2, 8, 128], F32); b_crep = fw.buf("crep")
        for j in range(2):
            fw.op("dve", lambda e: e.tensor_copy(crep[:, j], csil[:, :, j:j + 1].to_broadcast([128, 8, 128])),
                  reads=[b_csil], writes=[b_crep])
        abT = S(es, "abT", [128, 2, 48], F32); b_abT = fw.buf("abT", d_c)
        fw.dma("sp", abT[:], ada_bT, writes=[b_abT])
        ngT = S(es, "ngT", [128, 2, 2, 8], F32); b_ngT = fw.buf("ngT", d_c)
        fw.dma("sp", ngT[:].rearrange("p a b k -> p (a b k)"), norm_gT, writes=[b_ngT])
        d_slab = [fw.dsem("aw0"), fw.dsem("aw1")]
        slabs = [S(es, "awslab%d" % i, [128, 8, D], F32) for i in range(2)]
        b_slab = [fw.buf("awslab%d" % i, d_slab[i]) for i in range(2)]
        d_bb = fw.dsem("bb")
        bb = S(es, "bb", [128, D], F32); b_bb = fw.buf("bb", d_bb)
        gtmp = S(es, "gtmp", [128, D], F32); b_gtmp = fw.buf("gtmp")
        n = 0
        for i in range(2):
            for v in range(6):
                sl, bsl = slabs[n % 2], b_slab[n % 2]
                n += 1
                for k in range(8):
                    fw.dma("sp", sl[:, k, :], ada_w[i, k * 128:(k + 1) * 128, v * D:(v + 1) * D], writes=[bsl])
                for m in range(8):
                    for k in range(8):
                        fw.op("pe", lambda e: e.matmul(ps[0][:, m * 2:m * 2 + 2], lhsT=sl[:, k, m * 128:(m + 1) * 128],
                                                       rhs=csil[:, k, :], start=(k == 0), stop=(k == 7)),
                              reads=[bsl, b_csil], writes=[b_ps[0]])
                fw.op("dve", lambda e: e.tensor_tensor(
                    modT[:, i, v * 8:(v + 1) * 8, :], ps[0][:, 0:16].rearrange("p (m j) -> p m j", j=2),
                    abT[:, i, v * 8:(v + 1) * 8].unsqueeze(2).to_broadcast([128, 8, 2]), ALU.add),
                    reads=[b_ps[0], b_abT], writes=[b_modT])
                if v in (2, 5) or (i == 1 and v in (3, 4)):
                    fw.dma("sp", bb[:], ada_b[i, v * D:(v + 1) * D].partition_broadcast(128), writes=[b_bb])
                    for j in range(2):
                        if (i == 1 and j == 1) or (v in (3, 4) and j == 1):
                            continue
                        for s in range(2):
                            for k in range(8):
                                fw.op("pe", lambda e: e.matmul(ps[1 + s][:], lhsT=crep[:, j, k, :],
                                                               rhs=sl[:, k, s * 512:(s + 1) * 512],
                                                               start=(k == 0), stop=(k == 7)),
                                      reads=[bsl, b_crep], writes=[b_ps[1 + s]])
                            fw.op("dve", lambda e: e.tensor_tensor(gtmp[:, s * 512:(s + 1) * 512], ps[1 + s][:],
                                                                   bb[:, s * 512:(s + 1) * 512], ALU.add),
                                  reads=[b_ps[1 + s], b_bb], writes=[b_gtmp])
                        gi = {(0, 2, 0): 0, (0, 5, 0): 1, (0, 2, 1): 2, (0, 5, 1): 3, (1, 2, 0): 4, (1, 5, 0): 5,
                              (1, 3, 0): 6, (1, 4, 0): 7}[(i, v, j)]
                        fw.dma("sp", gb_d[gi], gtmp[:], reads=[b_gtmp], writes=[b_gb])
        for i in range(2):
            for nn in range(2):
                scv = 1 + 3 * nn
                for j in range(2):
                    fw.op("dve", lambda e: e.scalar_tensor_tensor(
                        scl[:, i, nn, j, :], modT[:, i, scv * 8:(scv + 1) * 8, j], 1.0, ngT[:, i, nn, :],
                        ALU.add, ALU.mult), reads=[b_modT, b_ngT], writes=[b_scl])
        fw.barrier()

    def shift_ap(i, nn, j, k):
        shv = 3 * nn
        return modT[:, i, shv * 8 + k, j:j + 1]

    def norm_transpose(hx_ap, b_hx, i, nn, j, dst_fn, b_dst, pb=(0, 1), pb_alt=None):
        par = nnt[0] % 2
        nnt[0] += 1
        ss, b_ss, xs, b_xs = ss2[par], b_ss2[par], xs2[par], b_xs2[par]
        if pb_alt is not None and par == 1:
            pb = pb_alt
        fw.op("dve", lambda e: e.memset(ss[:, 0:1], 0.0), writes=[b_ss])
        fw.op("act", lambda e: e.activation(junk[:], hx_ap, AF.Square, accum_out=ss[:, 0:1]),
              reads=[b_hx], writes=[b_ss])
        fw.op("act", lambda e: e.activation(ss[:, 1:2], ss[:, 0:1], AF.Sqrt, bias=eps_t[:, 0:1], scale=1.0 / D),
              reads=[b_ss, b_eps], writes=[b_ss])
        fw.op("dve", lambda e: e.reciprocal(ss[:, 2:3], ss[:, 1:2]), reads=[b_ss], writes=[b_ss])
        fw.op("dve", lambda e: e.tensor_scalar(xs[:], hx_ap, ss[:, 2:3], None, ALU.mult),
              reads=[b_hx, b_ss], writes=[b_xs])
        halves = [range(0, 8)] if pb[0] != pb[1] else [range(0, 4), range(4, 8)]
        for ks in halves:
            for k in ks:
                bank = pb[k // 4]
                fw.op("pe", lambda e: e.transpose(ps[bank][:, (k % 4) * 128:(k % 4 + 1) * 128],
                                                  xs[:, k * 128:(k + 1) * 128], ident[:]),
                      reads=[b_xs, b_ident], writes=[b_ps[bank]])
            for k in ks:
                bank = pb[k // 4]
                fw.op("act", lambda e: e.activation(dst_fn(k), ps[bank][:, (k % 4) * 128:(k % 4 + 1) * 128],
                                                    AF.Identity, bias=shift_ap(i, nn, j, k), scale=scl[:, i, nn, j, k:k + 1]),
                      reads=[b_ps[bank], b_scl, b_modT], writes=[b_dst])
        return xs, b_xs

    def chunk_src(ci):
        if ci < 34:
            return x_loc[ci * 128:(ci + 1) * 128, :]
        return ctx_b[(ci - 34) * 128:(ci - 33) * 128, :]

    def modset(ci):
        return 1 if ci >= 34 else 0

    if stop_after >= 1:
        with ExitStack() as es:
            d_p1 = fw.dsem("p1w")
            wu_sb = S(es, "wu_sb", [128, 8, D], BF16)
            wv_sb = S(es, "wv_sb", [128, 8, D], BF16)
            wo_sb = S(es, "wo_sb", [128, 8, D], BF16)
            b_wsb = fw.buf("p1w", d_p1)
            for k in range(8):
                fw.dma("sp", wu_sb[:, k, :], w_in_b[k * 128:(k + 1) * 128, 0:D], reads=[b_w0], writes=[b_wsb])
                fw.dma("sp", wv_sb[:, k, :], w_in_b[k * 128:(k + 1) * 128, D:2 * D], reads=[b_w0], writes=[b_wsb])
                fw.dma("sp", wo_sb[:, k, :], w_aout_b[k * 128:(k + 1) * 128, :], reads=[b_w0], writes=[b_wsb])
            wsp = S(es, "wsp", [128, 16, 128], F32); b_wsp = fw.buf("wsp", d_p1)
            fw.dma("sp", wsp[:], a_w_s.rearrange("g p q -> p g q"), writes=[b_wsp])
            bsT = S(es, "bsT", [128, 16], F32); b_bs = fw.buf("bs", d_p1)
            fw.dma("sp", bsT[:], a_b_sT, writes=[b_bs])
            vg_bc = S(es, "vg_bc", [128, D], F32); vb_bc = S(es, "vb_bc", [128, D], F32)
            b_vgb = fw.buf("vgb", d_p1)
            fw.dma("sp", vg_bc[:], a_v_g.partition_broadcast(128), writes=[b_vgb])
            fw.dma("sp", vb_bc[:], a_v_b.partition_broadcast(128), writes=[b_vgb])
            g1 = S(es, "g1", [128, 2, D], F32); b_g1 = fw.buf("g1", d_p1)
            fw.dma("sp", g1[:, 0, :], gb_d[0], reads=[b_gb], writes=[b_g1])
            fw.dma("sp", g1[:, 1, :], gb_d[2], reads=[b_gb], writes=[b_g1])
            wsT = S(es, "wsT", [128, 16, 128], BF16); b_wsT = fw.buf("wsT")
            for g in range(16):
                bk = g % 2
                fw.op("pe", lambda e: e.transpose(ps[bk][:, 0:128], wsp[:, g, :], ident[:]),
                      reads=[b_wsp, b_ident], writes=[b_ps[bk]])
                fw.op("act", lambda e: e.copy(wsT[:, g, :], ps[bk][:, 0:128]), reads=[b_ps[bk]], writes=[b_wsT])
            idb1 = S(es, "idb1", [128, 128], BF16); b_idb1 = fw.buf("idb1")
            fw.op("dve", lambda e: e.tensor_copy(idb1[:], ident[:]), reads=[b_ident], writes=[b_idb1])

            d_hx = [fw.dsem("hx0"), fw.dsem("hx1")]
            hx = [S(es, "hx%d" % i, [128, 4, D], F32) for i in range(2)]
            b_hx = [[fw.buf("hx%d_%d" % (i, c), d_hx[i]) for c in range(4)] for i in range(2)]
            xnT = S(es, "xnT", [128, 8, 512], BF16); b_xnT = fw.bufs("xnT", 4)
            uf2 = [S(es, "uf%d" % i, [128, D], F32) for i in range(2)]; b_uf2 = fw.bufs("uf", 2)
            vf2 = [S(es, "vf%d" % i, [128, D], F32) for i in range(2)]; b_vf2 = fw.bufs("vf", 2)
            vbf2 = [S(es, "vbf%d" % i, [128, D], BF16) for i in range(2)]; b_vbf2 = fw.bufs("vbf", 2)
            bn2 = [S(es, "bn%d" % i, [128, 2, 6], F32) for i in range(2)]
            mv2 = [S(es, "mv%d" % i, [128, 4], F32) for i in range(2)]; b_bn2 = fw.bufs("bn", 2)
            stmp2 = [S(es, "stmp%d" % i, [128, 512], F32) for i in range(2)]; b_stmp2 = fw.bufs("stmp", 2)
            ttok2 = [S(es, "ttok%d" % i, [128, D], BF16) for i in range(2)]; b_ttok2 = fw.bufs("ttok", 2)
            tT82 = [S(es, "tT8_%d" % i, [128, 8, 128], BF16) for i in range(2)]; b_tT82 = fw.bufs("tT8", 2)
            otmp2 = [S(es, "otmp%d" % i, [128, 512], F32) for i in range(2)]; b_otmp2 = fw.bufs("otmp", 2)
            psb0 = ps[0][:].bitcast(BF16)

            tiles = [list(range(t * 4, t * 4 + 4)) for t in range(8)] + [[32, 33], [34, 35]]

            def load_tile1(ti):
                if ti >= len(tiles):
                    return
                for c, ci in enumerate(tiles[ti]):
                    fw.dma("sp", hx[ti % 2][:, c, :], chunk_src(ci), writes=[b_hx[ti % 2][c]])

            xnT_1b = S(es, "xnT1b", [128, 8, 512], BF16); b_xnT_1b = fw.bufs("xnT1b", 4)
            xn1 = [(xnT, b_xnT), (xnT_1b, b_xnT_1b)]

            def norm_tile1(ti_):
                if ti_ >= len(tiles):
                    return
                xn_, bxn_ = xn1[ti_ % 2]
                for c_, ci_ in enumerate(tiles[ti_]):
                    norm_transpose(hx[ti_ % 2][:, c_, :], b_hx[ti_ % 2][c_], 0, 0, modset(tiles[ti_][0]),
                                   lambda k: xn_[:, k, c_ * 128:(c_ + 1) * 128], bxn_[c_])

            load_tile1(0)
            norm_tile1(0)
            for ti, chunks in enumerate(tiles):
                nt = len(chunks) * 128
                hb, bhb = hx[ti % 2], b_hx[ti % 2]
                j = modset(chunks[0])
                load_tile1(ti + 1)
                pump_casts(6)
                xnT, b_xnT = xn1[ti % 2]

                def stage_a(c):
                    b2 = c % 2
                    for (w_, dstt, b_dstt, pbase) in ((wu_sb, uf2[b2], b_uf2[b2], 2), (wv_sb, vf2[b2], b_vf2[b2], 4)):
                        for s in range(2):
                            for k in range(8):
                                fw.op("pe", lambda e: e.matmul(ps[pbase + s][:], lhsT=xnT[:, k, c * 128:(c + 1) * 128],
                                                               rhs=w_[:, k, s * 512:(s + 1) * 512],
                                                               start=(k == 0), stop=(k == 7)),
                                      reads=[b_wsb, b_xnT[c]], writes=[b_ps[pbase + s]])
                            fw.op("act", lambda e: e.activation(dstt[:, s * 512:(s + 1) * 512], ps[pbase + s][:], AF.Gelu_apprx_tanh),
                                  reads=[b_ps[pbase + s]], writes=[b_dstt])
                    for s in range(2):
                        fw.op("dve", lambda e: e.bn_stats(bn2[b2][:, s, :], vf2[b2][:, s * 512:(s + 1) * 512]),
                              reads=[b_vf2[b2]], writes=[b_bn2[b2]])
                    fw.op("dve", lambda e: e.bn_aggr(mv2[b2][:, 0:2], bn2[b2][:]), reads=[b_bn2[b2]], writes=[b_bn2[b2]])
                    fw.op("act", lambda e: e.activation(mv2[b2][:, 2:3], mv2[b2][:, 1:2], AF.Sqrt, bias=eps_t[:, 0:1], scale=1.0),
                          reads=[b_bn2[b2], b_eps], writes=[b_bn2[b2]])
                    fw.op("dve", lambda e: e.reciprocal(mv2[b2][:, 3:4], mv2[b2][:, 2:3]), reads=[b_bn2[b2]], writes=[b_bn2[b2]])
                    fw.op("dve", lambda e: e.tensor_scalar(vf2[b2][:], vf2[b2][:], mv2[b2][:, 0:1], mv2[b2][:, 3:4],
                                                           ALU.subtract, ALU.mult),
                          reads=[b_vf2[b2], b_bn2[b2]], writes=[b_vf2[b2]])
                    fw.op("dve", lambda e: e.tensor_tensor(vf2[b2][:], vf2[b2][:], vg_bc[:], ALU.mult),
                          reads=[b_vf2[b2], b_vgb], writes=[b_vf2[b2]])
                    fw.op("dve", lambda e: e.tensor_tensor(vbf2[b2][:], vf2[b2][:], vb_bc[:], ALU.add),
                          reads=[b_vf2[b2], b_vgb], writes=[b_vbf2[b2]])

                def stage_b1(c):
                    b2 = c % 2
                    for g in range(16):
                        bk = 6 + g // 8
                        fw.op("pe", lambda e: e.matmul(ps[bk][:, (g % 8) * 64:(g % 8 + 1) * 64], lhsT=wsT[:, g, :],
                                                       rhs=vbf2[b2][:, g * 64:(g + 1) * 64], start=True, stop=True),
                              reads=[b_vbf2[b2], b_wsT], writes=[b_ps[bk]])
                    for hf in range(2):
                        bk = 6 + hf
                        fw.op("dve", lambda e: e.tensor_tensor(
                            stmp2[hf][:].rearrange("p (g c) -> p g c", g=8), ps[bk][:].rearrange("p (g c) -> p g c", g=8),
                            bsT[:, hf * 8:(hf + 1) * 8].unsqueeze(2).to_broadcast([128, 8, 64]), ALU.add),
                            reads=[b_ps[bk], b_bs], writes=[b_stmp2[hf]])
                        fw.op("dve", lambda e: e.tensor_tensor(ttok2[b2][:, hf * 512:(hf + 1) * 512], stmp2[hf][:],
                                                               uf2[b2][:, hf * 512:(hf + 1) * 512], ALU.mult),
                              reads=[b_stmp2[hf], b_uf2[b2]], writes=[b_ttok2[b2]])
                    for k in range(8):
                        fw.op("pe", lambda e: e.transpose(psb0[:, k * 128:(k + 1) * 128], ttok2[b2][:, k * 128:(k + 1) * 128], idb1[:]),
                              reads=[b_ttok2[b2], b_idb1], writes=[b_ps[0]])
                    fw.op("act", lambda e: e.copy(tT82[b2][:].rearrange("p k q -> p (k q)"), psb0[:, :]),
                          reads=[b_ps[0]], writes=[b_tT82[b2]])

                def stage_b2(c, ci):
                    b2 = c % 2
                    for s in range(2):
                        bk = (1, 3)[s]
                        for k in range(8):
                            fw.op("pe", lambda e: e.matmul(ps[bk][:], lhsT=tT82[b2][:, k, :],
                                                           rhs=wo_sb[:, k, s * 512:(s + 1) * 512],
                                                           start=(k == 0), stop=(k == 7)),
                                  reads=[b_tT82[b2], b_wsb], writes=[b_ps[bk]])
                        fw.op("dve", lambda e: e.tensor_tensor(otmp2[s][:], ps[bk][:], g1[:, j, s * 512:(s + 1) * 512], ALU.mult),
                              reads=[b_ps[bk], b_g1], writes=[b_otmp2[s]])
                        fw.op("dve", lambda e: e.tensor_tensor(hb[:, c, s * 512:(s + 1) * 512],
                                                                hb[:, c, s * 512:(s + 1) * 512], otmp2[s][:], ALU.add),
                              reads=[b_otmp2[s], bhb[c]], writes=[bhb[c]])
                    fw.dma("sp", hA[ci * 128:(ci + 1) * 128, :], hb[:, c, :], reads=[bhb[c]], writes=[b_hA])

                nch_t = len(chunks)
                stage_a(0)
                if nch_t > 1:
                    stage_a(1)
                stage_b1(0)
                norm_tile1(ti + 1)
                for c, ci in enumerate(chunks):
                    if c + 2 < nch_t:
                        stage_a(c + 2)
                    if c + 1 < nch_t:
                        stage_b1(c + 1)
                    stage_b2(c, ci)
            fw.barrier()

    def ffn_phase(es, layer, src, b_src, supers, experts, finalize):
        moe = layer == 1
        NSC = max(len(s) for s in supers)
        d_p = fw.dsem("f%dc" % layer)
        g2 = S(es, "g2", [128, 2, D], F32); b_g2 = fw.buf("g2", d_p)
        if moe:
            fw.dma("sp", g2[:, 0, :], gb_d[5], reads=[b_gb], writes=[b_g2])
            wr = S(es, "wr", [128, 8, NE], F32); b_wr = fw.buf("wr", d_p)
            fw.dma("sp", wr[:], moe_wr.rearrange("(k p) e -> p k e", p=128), writes=[b_wr])
            fgb = S(es, "fgb", [128, D], F32); b_fgb = fw.buf("fgb", d_p)
            fw.dma("sp", fgb[:], final_g.partition_broadcast(128), writes=[b_fgb])
            x32b = [S(es, "x32_%d" % i, [128, 8, 128], F32) for i in range(2)]; b_x32b = fw.bufs("x32", 2)
            lgb = [S(es, "lg%d" % i, [128, 8, NE], F32) for i in range(2)]; b_lgb = fw.bufs("lg", 2)
            smb = [S(es, "sm%d" % i, [128, 8], F32) for i in range(2)]
        else:
            fw.dma("sp", g2[:, 0, :], gb_d[1], reads=[b_gb], writes=[b_g2])
            fw.dma("sp", g2[:, 1, :], gb_d[3], reads=[b_gb], writes=[b_g2])
        gates2 = [S(es, "gates%d" % i, [128, NSC, NE], F32) for i in range(2)]
        b_gates2 = [fw.bufs("gates%d_" % i, NSC) for i in range(2)]
        xnT2 = [S(es, "fxnT%d" % i, [128, 8, NSC * 128], BF16) for i in range(2)]
        b_xnT2 = [fw.bufs("fxnT%d_" % i, NSC) for i in range(2)]
        acc = S(es, "acc", [128, NSC, D], F32); b_acc = fw.bufs("acc", NSC)
        d_hxp = [fw.dsem("fhxp0"), fw.dsem("fhxp1")]
        hxp = [S(es, "fhxp%d" % i, [128, D], F32) for i in range(2)]
        b_hxp = [fw.buf("fhxp%d" % i, d_hxp[i]) for i in range(2)]
        d_hxf0 = fw.dsem("fhxf0")
        _hxf = S(es, "fhxf", [128, D], F32); _bhxf = fw.buf("fhxf", d_hxf0)
        hxf = [_hxf, _hxf]; b_hxf = [_bhxf, _bhxf]
        d_sl = [fw.dsem("fsl0"), fw.dsem("fsl1")]
        slg = [S(es, "slg%d" % i, [128, 8, 512], BF16) for i in range(2)]
        slu = [S(es, "slu%d" % i, [128, 8, 512], BF16) for i in range(2)]
        sld = [S(es, "sld%d" % i, [128, 4, D], BF16) for i in range(2)]
        b_sl = [fw.buf("fsl%d" % i, d_sl[i]) for i in range(2)]
        sg = [S(es, "sg%d" % i, [128, 512], F32) for i in range(2)]; b_sg = fw.bufs("sg", 2)
        hid = [S(es, "hid%d" % i, [128, 4, 512], BF16) for i in range(2)]
        b_hid = [fw.bufs("hid%d_" % i, 4) for i in range(2)]
        _ft = S(es, "ftmp", [128, D], F32); _bft = fw.buf("ftmp")
        ftmp2 = [_ft, _ft]; b_ftmp2 = [_bft, _bft]

        prep_list = [(si, c) for si in range(len(supers)) for c in range(len(supers[si]))]
        prep_loaded = [0]
        prep_done = [0]

        def prep_load(n):
            if n >= len(prep_list) or n < prep_loaded[0]:
                return
            si_, c_ = prep_list[n]
            ci_ = supers[si_][c_]
            fw.dma("sp", hxp[n % 2][:], src[ci_ * 128:(ci_ + 1) * 128, :], reads=[b_src], writes=[b_hxp[n % 2]])
            prep_loaded[0] = n + 1

        def prep_next():
            n = prep_done[0]
            if n >= len(prep_list):
                return
            prep_done[0] = n + 1
            si, c = prep_list[n]
            ci = supers[si][c]
            xb = si % 2
            xnT, b_xnT, gates, b_gates = xnT2[xb], b_xnT2[xb], gates2[xb], b_gates2[xb]
            j = modset(ci)
            prep_load(n)
            hb, bhb = hxp[n % 2], b_hxp[n % 2]
            prep_load(n + 1)
            if not moe:
                norm_transpose(hb[:], bhb, layer, 1, j, lambda k: xnT[:, k, c * 128:(c + 1) * 128], b_xnT[c], pb=(7, 7))
                return
            x32, b_x32 = x32b[n % 2], b_x32b[n % 2]
            lg, b_lg, sm = lgb[n % 2], b_lgb[n % 2], smb[n % 2]
            norm_transpose(hb[:], bhb, layer, 1, j, lambda k: x32[:, k, :], b_x32, pb=(7, 7))
            fw.op("pool", lambda e: e.tensor_copy(xnT[:, :, c * 128:(c + 1) * 128], x32[:]),
                  reads=[b_x32], writes=[b_xnT[c]])
            for k in range(8):
                fw.op("pe", lambda e: e.matmul(ps[7][:, 0:NE], lhsT=x32[:, k, :], rhs=wr[:, k, :],
                                               start=(k == 0), stop=(k == 7)),
                      reads=[b_x32, b_wr], writes=[b_ps[7]])
            L = lg[:, 0, :]
            fw.op("dve", lambda e: e.tensor_copy(L, ps[7][:, 0:NE]), reads=[b_ps[7]], writes=[b_lg])
            fw.op("dve", lambda e: e.reduce_max(sm[:, 0:1], L, axis=AX.X), reads=[b_lg], writes=[b_lg])
            fw.op("dve", lambda e: e.tensor_scalar(lg[:, 1, :], L, sm[:, 0:1], None, ALU.is_equal),
                  reads=[b_lg], writes=[b_lg])
            fw.op("dve", lambda e: e.scalar_tensor_tensor(lg[:, 2, :], lg[:, 1, :], -1e30, L, ALU.mult, ALU.add),
                  reads=[b_lg], writes=[b_lg])
            fw.op("dve", lambda e: e.reduce_max(sm[:, 1:2], lg[:, 2, :], axis=AX.X), reads=[b_lg], writes=[b_lg])
            fw.op("dve", lambda e: e.tensor_scalar(lg[:, 3, :], L, sm[:, 1:2], None, ALU.is_ge),
                  reads=[b_lg], writes=[b_lg])
            fw.op("dve", lambda e: e.tensor_scalar(sm[:, 2:3], sm[:, 0:1], -1.0, None, ALU.mult),
                  reads=[b_lg], writes=[b_lg])
            fw.op("act", lambda e: e.activation(lg[:, 4, :], L, AF.Exp, bias=sm[:, 2:3], scale=1.0),
                  reads=[b_lg], writes=[b_lg])
            fw.op("dve", lambda e: e.tensor_tensor(lg[:, 5, :], lg[:, 4, :], lg[:, 3, :], ALU.mult),
                  reads=[b_lg], writes=[b_lg])
            fw.op("dve", lambda e: e.reduce_sum(sm[:, 3:4], lg[:, 5, :], axis=AX.X), reads=[b_lg], writes=[b_lg])
            fw.op("dve", lambda e: e.reciprocal(sm[:, 4:5], sm[:, 3:4]), reads=[b_lg], writes=[b_lg])
            fw.op("dve", lambda e: e.tensor_scalar(gates[:, c, :], lg[:, 5, :], sm[:, 4:5], None, ALU.mult),
                  reads=[b_lg], writes=[b_gates[c]])

        seq = [(si, ex, hs) for si in range(len(supers)) for ex in experts for hs in range(NHS)]
        slab_loaded = [0]

        def load_slab(n):
            if n >= len(seq) or n < slab_loaded[0]:
                return
            _, ex_, hs_ = seq[n]
            si_ = n % 2
            fw.dma("sp", slg[si_][:].rearrange("p k n -> p (k n)"), wg_b[ex_, hs_], reads=[b_wf[ex_]], writes=[b_sl[si_]])
            fw.dma("sp", slu[si_][:].rearrange("p k n -> p (k n)"), wu_b[ex_, hs_], reads=[b_wf[ex_]], writes=[b_sl[si_]])
            fw.dma("sp", sld[si_][:].rearrange("p f n -> p (f n)"), wd_b[ex_, hs_], reads=[b_wf[ex_]], writes=[b_sl[si_]])
            slab_loaded[0] = n + 1

        nfin = 0
        nhid = 0
        nslab = 0
        load_slab(0)
        for _ in range(len(supers[0])):
            prep_next()
        for si, chunks in enumerate(supers):
            nsc = len(chunks)
            xb = si % 2
            xnT, b_xnT, gates, b_gates = xnT2[xb], b_xnT2[xb], gates2[xb], b_gates2[xb]
            n_next = len(supers[si + 1]) if si + 1 < len(supers) else 0
            nsteps = len(experts) * NHS
            per_step = -(-n_next // max(1, nsteps - 1))
            first = True
            ntiles = (nsc + 3) // 4
            for e_i, ex in enumerate(experts):
                for hs in range(NHS):
                    sl_i = nslab % 2
                    nslab += 1
                    load_slab(nslab)
                    pump_casts(6)
                    for t in range(ntiles):
                        tch = list(range(t * 4, min(nsc, t * 4 + 4)))
                        nt = len(tch) * 128
                        t0 = t * 512
                        hi = nhid % 2
                        nhid += 1
                        for f in range(4):
                            pg, pu = (0, 1) if f % 2 == 0 else (2, 3)
                            for k in range(8):
                                fw.op("pe", lambda e: e.matmul(ps[pg][:, 0:nt], lhsT=slg[sl_i][:, k, f * 128:(f + 1) * 128],
                                                               rhs=xnT[:, k, t0:t0 + nt], start=(k == 0), stop=(k == 7)),
                                      reads=[b_sl[sl_i]] + [b_xnT[c] for c in tch], writes=[b_ps[pg]])
                            for k in range(8):
                                fw.op("pe", lambda e: e.matmul(ps[pu][:, 0:nt], lhsT=slu[sl_i][:, k, f * 128:(f + 1) * 128],
                                                               rhs=xnT[:, k, t0:t0 + nt], start=(k == 0), stop=(k == 7)),
                                      reads=[b_sl[sl_i]] + [b_xnT[c] for c in tch], writes=[b_ps[pu]])
                            fw.op("act", lambda e: e.activation(sg[f % 2][:, 0:nt], ps[pg][:, 0:nt], AF.Silu),
                                  reads=[b_ps[pg]], writes=[b_sg[f % 2]])
                            fw.op("dve", lambda e: e.tensor_tensor(hid[hi][:, f, 0:nt], sg[f % 2][:, 0:nt], ps[pu][:, 0:nt], ALU.mult),
                                  reads=[b_sg[f % 2], b_ps[pu]], writes=[b_hid[hi][f]])
                        for cl, c in enumerate(tch):
                            for s in range(2):
                                pd = 4 + nd[0] % 3
                                nd[0] += 1
                                for f in range(4):
                                    fw.op("pe", lambda e: e.matmul(ps[pd][:], lhsT=hid[hi][:, f, cl * 128:(cl + 1) * 128],
                                                                   rhs=sld[sl_i][:, f, s * 512:(s + 1) * 512],
                                                                   start=(f == 0), stop=(f == 3)),
                                          reads=[b_hid[hi][f], b_sl[sl_i]], writes=[b_ps[pd]])
                                a_ap = acc[:, c, s * 512:(s + 1) * 512]
                                if moe:
                                    if first:
                                        fw.op("dve", lambda e: e.tensor_scalar(a_ap, ps[pd][:], gates[:, c, e_i:e_i + 1], None, ALU.mult),
                                              reads=[b_ps[pd], b_gates[c]], writes=[b_acc[c]])
                                    else:
                                        fw.op("dve", lambda e: e.scalar_tensor_tensor(a_ap, ps[pd][:], gates[:, c, e_i:e_i + 1], a_ap,
                                                                                       ALU.mult, ALU.add),
                                              reads=[b_ps[pd], b_gates[c], b_acc[c]], writes=[b_acc[c]])
                                else:
                                    if first:
                                        fw.op("dve", lambda e: e.tensor_copy(a_ap, ps[pd][:]), reads=[b_ps[pd]], writes=[b_acc[c]])
                                    else:
                                        fw.op("dve", lambda e: e.tensor_tensor(a_ap, ps[pd][:], a_ap, ALU.add),
                                              reads=[b_ps[pd], b_acc[c]], writes=[b_acc[c]])
                    first = False
                    for _ in range(per_step):
                        if prep_done[0] < sum(len(x) for x in supers[:si + 2]):
                            prep_next()
            while prep_done[0] < sum(len(x) for x in supers[:si + 2]):
                prep_next()
            def fin_load(c_, slot):
                if c_ >= len(chunks):
                    return
                ci_ = chunks[c_]
                fw.dma("sp", hxf[slot % 2][:], src[ci_ * 128:(ci_ + 1) * 128, :], reads=[b_src], writes=[b_hxf[slot % 2]])

            for c, ci in enumerate(chunks):
                j = modset(ci)
                fpar = nfin % 2
                hb, bhb = hxf[fpar], b_hxf[fpar]
                ftmp, b_ftmp = ftmp2[fpar], b_ftmp2[fpar]
                ss, b_ss = ss2[fpar], b_ss2[fpar]
                ot, bot = ftmp, b_ftmp
                fin_load(c, nfin)
                nfin += 1
                fw.op("pool", lambda e: e.tensor_tensor(ftmp[:], acc[:, c, :], g2[:, j, :], ALU.mult),
                      reads=[b_acc[c], b_g2], writes=[b_ftmp])
                if not moe:
                    fw.op("pool", lambda e: e.tensor_tensor(ot[:], ftmp[:], hb[:], ALU.add),
                          reads=[b_ftmp, bhb], writes=[bot])
                    fw.dma("sp", h1[ci * 128:(ci + 1) * 128, :], ot[:], reads=[bot], writes=[b_h1])
                else:
                    fw.op("pool", lambda e: e.tensor_tensor(ftmp[:], ftmp[:], hb[:], ALU.add),
                          reads=[b_ftmp, bhb], writes=[b_ftmp])
                    fw.op("dve", lambda e: e.memset(ss[:, 0:1], 0.0), writes=[b_ss])
                    fw.op("act", lambda e: e.activation(junk[:], ftmp[:], AF.Square, accum_out=ss[:, 0:1]),
                          reads=[b_ftmp], writes=[b_ss])
                    fw.op("act", lambda e: e.activation(ss[:, 1:2], ss[:, 0:1], AF.Sqrt, bias=eps_t[:, 0:1], scale=1.0 / D),
                          reads=[b_ss, b_eps], writes=[b_ss])
                    fw.op("dve", lambda e: e.reciprocal(ss[:, 2:3], ss[:, 1:2]), reads=[b_ss], writes=[b_ss])
                    fw.op("dve", lambda e: e.scalar_tensor_tensor(ot[:], ftmp[:], ss[:, 2:3], fgb[:], ALU.mult, ALU.mult),
                          reads=[b_ftmp, b_ss, b_fgb], writes=[bot])
                    fw.dma("sp", out_d[ci * 128:(ci + 1) * 128, :], ot[:], reads=[bot], writes=[b_out])

    nld_s = [0]
    nd = [0]

    if stop_after >= 2:
        with ExitStack() as es:
            supers = [list(range(0, 12)), list(range(12, 24)), list(range(24, 36))]
            ffn_phase(es, 0, hA, b_hA, supers, [0], None)
            fw.barrier()

    pump_casts(len(cast_jobs))
    if stop_after >= 3:
        with ExitStack() as es:
            d_p3 = fw.dsem("p3w")
            b_w3 = fw.buf("p3w", d_p3)
            b_wqs = fw.buf("wqs")
            d_cs3 = [fw.dsem("cs3_0"), fw.dsem("cs3_1")]
            cosT2 = [S(es, "cosT%d" % i, [64, 512], F32) for i in range(2)]
            sinT2 = [S(es, "sinT%d" % i, [64, 512], F32) for i in range(2)]
            b_cs32 = [fw.buf("cs3_%d" % i, d_cs3[i]) for i in range(2)]
            mk32 = S(es, "mk32", [128, 4, 128], F32); b_mk32 = fw.buf("mk32", d_p3)
            fw.dma("sp", mk32[:], masks_d, writes=[b_mk32])
            mk = S(es, "mk", [128, 4, 128], BF16); b_mk = fw.buf("mk")
            fw.op("dve", lambda e: e.tensor_copy(mk[:], mk32[:]), reads=[b_mk32], writes=[b_mk])
            snk = S(es, "snk", [128, 16], F32); b_snk = fw.buf("snk", d_p3)
            fw.dma("sp", snk[:], b_sink.partition_broadcast(128), writes=[b_snk])
            esnk = S(es, "esnk", [128, 16], F32); b_esnk = fw.buf("esnk")
            fw.op("act", lambda e: e.activation(esnk[:], snk[:], AF.Exp), reads=[b_snk], writes=[b_esnk])
            idb = S(es, "idb", [128, 128], BF16); b_idb = fw.buf("idb")
            fw.op("dve", lambda e: e.tensor_copy(idb[:], ident[:]), reads=[b_ident], writes=[b_idb])
            g1 = S(es, "g1b", [128, D], F32); b_g1 = fw.buf("g1b", d_p3)
            fw.dma("sp", g1[:], gb_d[4], reads=[b_gb], writes=[b_g1])

            kT = S(es, "kT", [64, 4, NCH * 128], BF16); b_kT = fw.bufs("kT", NCH)
            vA = S(es, "vA", [128, NCH, 4, 65], BF16); b_vA = fw.bufs("vA", NCH)
            fw.op("pool", lambda e: e.memset(vA[:, :, :, 64:65], 1.0), writes=b_vA)
            d_hx = [fw.dsem("ahx0"), fw.dsem("ahx1")]
            hx = [S(es, "ahx%d" % i, [128, 4, D], F32) for i in range(2)]
            b_hx = [[fw.buf("ahx%d_%d" % (i, c), d_hx[i]) for c in range(4)] for i in range(2)]
            xnT = S(es, "axnT", [128, 8, 512], BF16); b_xnT = fw.bufs("axnT", 4)
            r1 = S(es, "r1", [64, 512], F32); r2 = S(es, "r2", [64, 512], F32)
            b_r1 = fw.buf("r1"); b_r2 = fw.buf("r2")
            psb = ps[7][:].bitcast(BF16)

            def rope(dst_ap, b_dst, pa, pb_, slot, nt):
                fw.op("dve", lambda e: e.tensor_tensor(r1[:, 0:nt], ps[pa][0:64, 0:nt], cosT2[slot][:, 0:nt], ALU.mult),
                      reads=[b_ps[pa], b_cs32[slot]], writes=[b_r1])
                fw.op("dve", lambda e: e.tensor_tensor(r2[:, 0:nt], ps[pb_][0:64, 0:nt], sinT2[slot][:, 0:nt], ALU.mult),
                      reads=[b_ps[pb_], b_cs32[slot]], writes=[b_r2])
                fw.op("pool", lambda e: e.tensor_tensor(dst_ap, r1[:, 0:nt], r2[:, 0:nt], ALU.add),
                      reads=[b_r1, b_r2], writes=b_dst)

            esA = ExitStack()
            wk = S(esA, "wk", [128, 8, 256], BF16); wks = S(esA, "wks", [128, 8, 256], BF16)
            wv = S(esA, "wv", [128, 8, 256], BF16)
            for k in range(8):
                r = slice(k * 128, (k + 1) * 128)
                fw.dma("sp", wk[:, k, :], w_qkv_b[r, D:D + 256], reads=[b_w1], writes=[b_w3])
                fw.dma("sp", wv[:, k, :], w_qkv_b[r, D + 256:D + 512], reads=[b_w1], writes=[b_w3])

            def swap_halves(src_w, dst_w):
                sv = src_w[:].rearrange("p k (g h j) -> p (k g) h j", h=2, j=16)
                dv = dst_w[:].rearrange("p k (g h j) -> p (k g) h j", h=2, j=16)
                fw.op("pool", lambda e: e.tensor_copy(dv[:, :, 0, :], sv[:, :, 1, :]), reads=[b_w3], writes=[b_wqs])
                fw.op("pool", lambda e: e.tensor_copy(dv[:, :, 1, :], sv[:, :, 0, :]), reads=[b_w3], writes=[b_wqs])

            swap_halves(wk, wks)
            tiles = [list(range(t * 4, t * 4 + 4)) for t in range(8)] + [[32, 33], [34, 35]]
            nrot = 0
            tseq = [(ti, 0) for ti in range(10)] + [(ti, 1) for ti in range(8)]

            def load_tile3(n):
                if n >= len(tseq):
                    return
                ti_ = tseq[n][0]
                chunks_ = tiles[ti_]
                nt_ = len(chunks_) * 128
                lo_ = chunks_[0] * 128
                for c_, ci_ in enumerate(chunks_):
                    fw.dma("sp", hx[n % 2][:, c_, :], h1[ci_ * 128:(ci_ + 1) * 128, :], reads=[b_h1], writes=[b_hx[n % 2][c_]])
                fw.dma("sp", cosT2[n % 2][:, 0:nt_], cos_d[:, lo_:lo_ + nt_], writes=[b_cs32[n % 2]])
                fw.dma("sp", sinT2[n % 2][:, 0:nt_], sin_d[:, lo_:lo_ + nt_], writes=[b_cs32[n % 2]])

            xnTb = S(esA, "axnTb", [128, 8, 512], BF16); b_xnTb = fw.bufs("axnTb", 4)
            xnA = [(xnT, b_xnT), (xnTb, b_xnTb)]

            def norm_tile_a(ti_):
                if ti_ >= len(tiles):
                    return
                xn_, bxn_ = xnA[ti_ % 2]
                for c_, ci_ in enumerate(tiles[ti_]):
                    norm_transpose(hx[ti_ % 2][:, c_, :], b_hx[ti_ % 2][c_], 1, 0, modset(tiles[ti_][0]),
                                   lambda k: xn_[:, k, c_ * 128:(c_ + 1) * 128], bxn_[c_])

            load_tile3(0)
            norm_tile_a(0)
            for ti, chunks in enumerate(tiles):
                nt = len(chunks) * 128
                t_lo = ti % 2
                hb, bhb = hx[ti % 2], b_hx[ti % 2]
                j = modset(chunks[0])
                load_tile3(ti + 1)
                norm_tile_a(ti + 1)
                xnT_a, b_xnT_a = xnA[ti % 2]
                for kv in range(4):
                    pa, pb_ = (2, 3) if nrot % 2 == 0 else (4, 5)
                    nrot += 1
                    for (w_, p_) in ((wk, pa), (wks, pb_)):
                        for k in range(8):
                            fw.op("pe", lambda e: e.matmul(ps[p_][0:64, 0:nt], lhsT=w_[:, k, kv * 64:(kv + 1) * 64],
                                                           rhs=xnT_a[:, k, 0:nt], start=(k == 0), stop=(k == 7)),
                                  reads=[b_w3, b_wqs] + b_xnT_a[:len(chunks)], writes=[b_ps[p_]])
                    rope(kT[:, kv, chunks[0] * 128:chunks[0] * 128 + nt], [b_kT[ci] for ci in chunks], pa, pb_, t_lo, nt)
                for c, ci in enumerate(chunks):
                    for k in range(8):
                        fw.op("pe", lambda e: e.matmul(ps[6][:, 0:256], lhsT=xnT_a[:, k, c * 128:(c + 1) * 128], rhs=wv[:, k, :],
                                                       start=(k == 0), stop=(k == 7)),
                              reads=[b_w3, b_xnT_a[c]], writes=[b_ps[6]])
                    fw.op("act", lambda e: e.copy(vA[:, ci, :, 0:64], ps[6][:, 0:256].rearrange("p (h d) -> p h d", h=4)),
                          reads=[b_ps[6]], writes=[b_vA[ci]])
            fw.barrier()
            esA.close()
            wq = S(es, "wq", [128, 8, D], BF16); wqs = S(es, "wqs", [128, 8, D], BF16)
            wo = S(es, "wo", [128, 8, D], BF16)
            for k in range(8):
                r = slice(k * 128, (k + 1) * 128)
                fw.dma("sp", wq[:, k, :], w_qkv_b[r, 0:D], reads=[b_w1], writes=[b_w3])
                fw.dma("sp", wo[:, k, :], w_bout_b[r, :], reads=[b_w1], writes=[b_w3])
            swap_halves(wq, wqs)
            qT = S(es, "qT", [64, 16, 512], BF16); b_qT = fw.bufs("qT", 16)
            pT = [S(es, "pT%d" % i, [128, 512], BF16) for i in range(10)]; b_pT = fw.bufs("pT", 10)
            den2 = [S(es, "den%d" % i, [128, 8], F32) for i in range(2)]; b_den2 = fw.bufs("den", 2)
            otok2 = [S(es, "otok%d" % i, [128, D], BF16) for i in range(2)]; b_otok2 = fw.bufs("otok", 2)
            oT = S(es, "oT", [128, 8, 128], BF16); b_oT = fw.buf("oT")
            _ot = S(es, "aotmp", [128, 512], F32); _bot = fw.buf("aotmp")
            otmp2 = [_ot, _ot]; b_otmp2 = [_bot, _bot]
            npt_box = [0]
            for ti in range(8):
                chunks = tiles[ti]
                nt = 512
                n3 = 10 + ti
                t_lo = n3 % 2
                hb, bhb = hx[n3 % 2], b_hx[n3 % 2]
                load_tile3(n3 + 1)
                for c, ci in enumerate(chunks):
                    norm_transpose(hb[:, c, :], bhb[c], 1, 0, 0, lambda k: xnT[:, k, c * 128:(c + 1) * 128], b_xnT[c])
                for h in range(16):
                    pa, pb_ = (2, 3) if nrot % 2 == 0 else (4, 5)
                    nrot += 1
                    for (w_, p_) in ((wq, pa), (wqs, pb_)):
                        for k in range(8):
                            fw.op("pe", lambda e: e.matmul(ps[p_][0:64, 0:nt], lhsT=w_[:, k, h * 64:(h + 1) * 64],
                                                           rhs=xnT[:, k, 0:nt], start=(k == 0), stop=(k == 7)),
                                  reads=[b_w3, b_wqs] + b_xnT, writes=[b_ps[p_]])
                    rope(qT[:, h, :], [b_qT[h]], pa, pb_, t_lo, nt)
                def stage_s(c, ci, kv):
                    nonlocal_npt = npt_box
                    kprev = ci - 1 if ci > 0 else 32
                    knext = ci + 1 if ci < 31 else 33
                    mprev = 0 if ci > 0 else 2
                    mnext = 1 if ci < 31 else 3
                    klist = [(kprev, mprev), (ci, None), (knext, mnext), (34, None), (35, None)]
                    pts = []
                    for (kc, mi) in klist:
                        pi = nonlocal_npt[0] % 10
                        nonlocal_npt[0] += 1
                        bk = 2 + nonlocal_npt[0] % 3
                        fw.op("pe", lambda e: e.matmul(ps[bk][:], lhsT=kT[:, kv, kc * 128:(kc + 1) * 128],
                                                       rhs=qT[:, kv * 4:(kv + 1) * 4, c * 128:(c + 1) * 128],
                                                       start=True, stop=True),
                              reads=[b_kT[kc]] + b_qT[kv * 4:(kv + 1) * 4], writes=[b_ps[bk]])
                        fw.op("act", lambda e: e.activation(pT[pi][:], ps[bk][:], AF.Exp, scale=0.125),
                              reads=[b_ps[bk]], writes=[b_pT[pi]])
                        if mi is not None:
                            fw.op("pool", lambda e: e.tensor_tensor(
                                pT[pi][:].rearrange("p (h q) -> p h q", h=4), pT[pi][:].rearrange("p (h q) -> p h q", h=4),
                                mk[:, mi:mi + 1, :].to_broadcast([128, 4, 128]), ALU.mult),
                                reads=[b_pT[pi], b_mk], writes=[b_pT[pi]])
                        pts.append((pi, kc))
                    return pts

                def stage_pv(c, kv, pts):
                    po = 5 + kv % 2
                    ob = c % 2
                    for hh in range(4):
                        for n_, (pi, kc) in enumerate(pts):
                            fw.op("pe", lambda e: e.matmul(ps[po][:, hh * 65:(hh + 1) * 65], lhsT=pT[pi][:, hh * 128:(hh + 1) * 128],
                                                           rhs=vA[:, kc, kv, :], start=(n_ == 0), stop=(n_ == 4)),
                                  reads=[b_pT[pi], b_vA[kc]], writes=[b_ps[po]])
                    pov = ps[po][:, 0:260].rearrange("p (h d) -> p h d", h=4)
                    dn = den2[kv % 2]
                    fw.op("dve", lambda e: e.tensor_tensor(dn[:, 0:4], pov[:, :, 64], esnk[:, kv * 4:(kv + 1) * 4], ALU.add),
                          reads=[b_ps[po], b_esnk], writes=[b_den2[kv % 2]])
                    fw.op("dve", lambda e: e.reciprocal(dn[:, 4:8], dn[:, 0:4]), reads=[b_den2[kv % 2]], writes=[b_den2[kv % 2]])
                    fw.op("dve", lambda e: e.tensor_tensor(
                        otok2[ob][:, kv * 256:(kv + 1) * 256].rearrange("p (h d) -> p h d", h=4), pov[:, :, 0:64],
                        dn[:, 4:8].unsqueeze(2).to_broadcast([128, 4, 64]), ALU.mult),
                        reads=[b_ps[po], b_den2[kv % 2]], writes=[b_otok2[ob]])

                def stage_out(c, ci):
                    ob = c % 2
                    for k in range(8):
                        fw.op("pe", lambda e: e.transpose(psb[:, k * 128:(k + 1) * 128], otok2[ob][:, k * 128:(k + 1) * 128], idb[:]),
                              reads=[b_otok2[ob], b_idb], writes=[b_ps[7]])
                    fw.op("act", lambda e: e.copy(oT[:].rearrange("p k q -> p (k q)"), psb[:, :]), reads=[b_ps[7]], writes=[b_oT])
                    for s in range(2):
                        bk = s
                        for k in range(8):
                            fw.op("pe", lambda e: e.matmul(ps[bk][:], lhsT=oT[:, k, :], rhs=wo[:, k, s * 512:(s + 1) * 512],
                                                           start=(k == 0), stop=(k == 7)),
                                  reads=[b_oT, b_w3], writes=[b_ps[bk]])
                        fw.op("dve", lambda e: e.tensor_tensor(otmp2[s][:], ps[bk][:], g1[:, s * 512:(s + 1) * 512], ALU.mult),
                              reads=[b_ps[bk], b_g1], writes=[b_otmp2[s]])
                        fw.op("pool", lambda e: e.tensor_tensor(hb[:, c, s * 512:(s + 1) * 512],
                                                                hb[:, c, s * 512:(s + 1) * 512], otmp2[s][:], ALU.add),
                              reads=[b_otmp2[s], bhb[c]], writes=[bhb[c]])
                    fw.dma("sp", hB[ci * 128:(ci + 1) * 128, :], hb[:, c, :], reads=[bhb[c]], writes=[b_hB])

                items = [(c, ci, kv) for c, ci in enumerate(chunks) for kv in range(4)]
                pend = None
                for (c, ci, kv) in items:
                    pts = stage_s(c, ci, kv)
                    if pend is not None:
                        stage_pv(pend[0], pend[2], pend[3])
                        if pend[2] == 3:
                            stage_out(pend[0], pend[1])
                    pend = (c, ci, kv, pts)
                stage_pv(pend[0], pend[2], pend[3])
                stage_out(pend[0], pend[1])
            fw.barrier()


    I32 = mybir.dt.int32
    IOA = bass.IndirectOffsetOnAxis

    def moe_sparse_phase():
        NT = 23
        with ExitStack() as es:
            d_c4 = fw.dsem("p4c")
            g2r = S(es, "g2r", [128, D], F32); b_g2r = fw.buf("g2r", d_c4)
            fw.dma("sp", g2r[:], gb_d[5], reads=[b_gb], writes=[b_g2r])
            fgb = S(es, "fgb", [128, D], F32); b_fgb = fw.buf("fgb", d_c4)
            fw.dma("sp", fgb[:], final_g.partition_broadcast(128), writes=[b_fgb])
            s1i = S(es, "s1i", [128, 32], I32); s2i = S(es, "s2i", [128, 32], I32)
            w1 = S(es, "w1", [128, 32], F32); w2 = S(es, "w2", [128, 32], F32)
            widx = S(es, "widx", [128, NT * NHS], I32)
            b_idx = fw.buf("idx")

            esA = ExitStack()
            wr = S(esA, "wr", [128, 8, NE], F32); b_wr = fw.buf("wr", d_c4)
            fw.dma("sp", wr[:], moe_wr.rearrange("(k p) e -> p k e", p=128), writes=[b_wr])
            scr = S(esA, "scr", [128, D], F32); shr = S(esA, "shr", [128, D], F32); ngr = S(esA, "ngr", [128, D], F32)
            b_rows = fw.buf("rows", d_c4)
            fw.dma("sp", scr[:], gb_d[7], reads=[b_gb], writes=[b_rows])
            fw.dma("sp", shr[:], gb_d[6], reads=[b_gb], writes=[b_rows])
            fw.dma("sp", ngr[:], norm_g_raw[1, 1].partition_broadcast(128), writes=[b_rows])
            tri = S(esA, "tri", [128, 128], F32); b_tri = fw.buf("tri", d_c4)
            fw.dma("sp", tri[:], tri_d, writes=[b_tri])
            ones = S(esA, "ones", [128, 128], F32); b_ones = fw.buf("ones")
            fw.op("dve", lambda e: e.memset(ones[:], 1.0), writes=[b_ones])
            b_scr = fw.buf("scr")
            fw.op("dve", lambda e: e.scalar_tensor_tensor(scr[:], scr[:], 1.0, ngr[:], ALU.add, ALU.mult),
                  reads=[b_rows], writes=[b_scr])
            x32b = [S(esA, "x32_%d" % i, [128, 8, 128], F32) for i in range(2)]; b_x32b = fw.bufs("x32", 2)
            lgb = [S(esA, "lg%d" % i, [128, 8, NE], F32) for i in range(2)]; b_lgb = fw.bufs("lg", 2)
            smb = [S(esA, "sm%d" % i, [128, 8], F32) for i in range(2)]
            Msel = S(esA, "Msel", [128, 32, NE], F32); M1 = S(esA, "M1", [128, 32, NE], F32)
            gates = S(esA, "gates", [128, 32, NE], F32); b_M = fw.buf("M")
            xn_all = S(esA, "xn_all", [128, 32, D], BF16); b_xn = fw.bufs("xn", 32)
            xt2 = [S(esA, "xt%d" % i, [128, D], F32) for i in range(2)]; b_xt2 = fw.bufs("xt", 2)
            d_hxp = [fw.dsem("shxp0"), fw.dsem("shxp1")]
            hxp = [S(esA, "shxp%d" % i, [128, D], F32) for i in range(2)]
            b_hxp = [fw.buf("shxp%d" % i, d_hxp[i]) for i in range(2)]

            def ld(c):
                if c < 32:
                    fw.dma("sp", hxp[c % 2][:], hB[c * 128:(c + 1) * 128, :], reads=[b_hB], writes=[b_hxp[c % 2]])

            def stage_n(c):
                hb, bhb = hxp[c % 2], b_hxp[c % 2]
                ld(c + 1)
                x32, b_x32 = x32b[c % 2], b_x32b[c % 2]
                xs_, b_xs_ = norm_transpose(hb[:], bhb, 1, 1, 0, lambda k: x32[:, k, :], b_x32, pb=(7, 7))
                xt, b_xt = xt2[c % 2], b_xt2[c % 2]
                fw.op("pool", lambda e: e.tensor_tensor(xt[:], xs_[:], scr[:], ALU.mult), reads=[b_xs_, b_scr], writes=[b_xt])
                fw.op("pool", lambda e: e.tensor_tensor(xn_all[:, c, :], xt[:], shr[:], ALU.add),
                      reads=[b_xt, b_rows], writes=[b_xn[c]])

            ld(0)
            stage_n(0)
            for c in range(32):
                if c + 1 < 32:
                    stage_n(c + 1)
                x32, b_x32 = x32b[c % 2], b_x32b[c % 2]
                lg, b_lg, sm = lgb[c % 2], b_lgb[c % 2], smb[c % 2]
                for k in range(8):
                    fw.op("pe", lambda e: e.matmul(ps[6][:, 0:NE], lhsT=x32[:, k, :], rhs=wr[:, k, :],
                                                   start=(k == 0), stop=(k == 7)),
                          reads=[b_x32, b_wr], writes=[b_ps[6]])
                L = lg[:, 0, :]
                fw.op("dve", lambda e: e.tensor_copy(L, ps[6][:, 0:NE]), reads=[b_ps[6]], writes=[b_lg])
                fw.op("dve", lambda e: e.reduce_max(sm[:, 0:1], L, axis=AX.X), reads=[b_lg], writes=[b_lg])
                fw.op("dve", lambda e: e.tensor_scalar(M1[:, c, :], L, sm[:, 0:1], None, ALU.is_equal),
                      reads=[b_lg], writes=[b_M])
                fw.op("dve", lambda e: e.scalar_tensor_tensor(lg[:, 2, :], M1[:, c, :], -1e30, L, ALU.mult, ALU.add),
                      reads=[b_lg, b_M], writes=[b_lg])
                fw.op("dve", lambda e: e.reduce_max(sm[:, 1:2], lg[:, 2, :], axis=AX.X), reads=[b_lg], writes=[b_lg])
                fw.op("dve", lambda e: e.tensor_scalar(Msel[:, c, :], L, sm[:, 1:2], None, ALU.is_ge),
                      reads=[b_lg], writes=[b_M])
                fw.op("dve", lambda e: e.tensor_scalar(sm[:, 2:3], sm[:, 0:1], -1.0, None, ALU.mult),
                      reads=[b_lg], writes=[b_lg])
                fw.op("act", lambda e: e.activation(lg[:, 4, :], L, AF.Exp, bias=sm[:, 2:3], scale=1.0),
                      reads=[b_lg], writes=[b_lg])
                fw.op("dve", lambda e: e.tensor_tensor(lg[:, 5, :], lg[:, 4, :], Msel[:, c, :], ALU.mult),
                      reads=[b_lg, b_M], writes=[b_lg])
                fw.op("dve", lambda e: e.reduce_sum(sm[:, 3:4], lg[:, 5, :], axis=AX.X), reads=[b_lg], writes=[b_lg])
                fw.op("dve", lambda e: e.reciprocal(sm[:, 4:5], sm[:, 3:4]), reads=[b_lg], writes=[b_lg])
                fw.op("dve", lambda e: e.tensor_scalar(gates[:, c, :], lg[:, 5, :], sm[:, 4:5], None, ALU.mult),
                      reads=[b_lg], writes=[b_M])

            CE = 32 * NE
            Mflat = Msel[:].rearrange("p c e -> p (c e)")
            fw.op("pe", lambda e: e.matmul(ps[0][:, 0:CE], lhsT=tri[:], rhs=Mflat, start=True, stop=True),
                  reads=[b_tri, b_M], writes=[b_ps[0]])
            fw.op("pe", lambda e: e.matmul(ps[1][:, 0:CE], lhsT=ones[:], rhs=Mflat, start=True, stop=True),
                  reads=[b_ones, b_M], writes=[b_ps[1]])
            P1 = S(esA, "P1", [128, 32, NE], F32); T = S(esA, "T", [128, 32, NE], F32)
            Cpre = S(esA, "Cpre", [128, 32, NE], F32); slot = S(esA, "slot", [128, 32, NE], F32)
            M2 = S(esA, "M2", [128, 32, NE], F32); tmp3 = S(esA, "tmp3", [128, 32, NE], F32)
            sml = S(esA, "sml", [128, 64], F32)
            s1f = S(esA, "s1f", [128, 32], F32); s2f = S(esA, "s2f", [128, 32], F32)
            tvi = S(esA, "tvi", [128, NT], I32); tv = S(esA, "tv", [128, NT], F32); et = S(esA, "et", [128, NT], F32)
            csti = S(esA, "csti", [128, NHS], I32); cst = S(esA, "cst", [128, NHS], F32)
            widxf = S(esA, "widxf", [128, NT, NHS], F32)
            b_B = fw.buf("B")
            V = lambda fn, r=(), w=(): fw.op("dve", fn, reads=list(r) + [b_B], writes=list(w) + [b_B])
            V(lambda e: e.tensor_copy(P1[:].rearrange("p c e -> p (c e)"), ps[0][:, 0:CE]), r=[b_ps[0]])
            V(lambda e: e.tensor_copy(T[:].rearrange("p c e -> p (c e)"), ps[1][:, 0:CE]), r=[b_ps[1]])
            V(lambda e: e.memset(Cpre[:, 0, :], 0.0))
            for c in range(1, 32):
                V(lambda e: e.tensor_tensor(Cpre[:, c, :], Cpre[:, c - 1, :], T[:, c - 1, :], ALU.add))
            cnt, nt8, tb, base = sml[:, 0:8], sml[:, 8:16], sml[:, 16:25], sml[:, 32:40]
            V(lambda e: e.tensor_tensor(cnt, Cpre[:, 31, :], T[:, 31, :], ALU.add))
            V(lambda e: e.tensor_scalar(nt8, cnt, 0.0, None, ALU.is_gt))
            for jj in range(1, 8):
                V(lambda e: e.scalar_tensor_tensor(nt8, cnt, 512.0 * jj, nt8, ALU.is_gt, ALU.add))
            V(lambda e: e.memset(sml[:, 16:17], 0.0))
            for ee in range(NE):
                V(lambda e: e.tensor_tensor(sml[:, 17 + ee:18 + ee], sml[:, 16 + ee:17 + ee], sml[:, 8 + ee:9 + ee], ALU.add))
            V(lambda e: e.tensor_scalar(base, sml[:, 16:24], 512.0, None, ALU.mult))
            V(lambda e: e.tensor_tensor(slot[:], P1[:], Cpre[:], ALU.add))
            V(lambda e: e.tensor_tensor(slot[:], slot[:], base.unsqueeze(1).to_broadcast([128, 32, NE]), ALU.add))
            V(lambda e: e.tensor_tensor(M2[:], Msel[:], M1[:], ALU.subtract), r=[b_M])
            V(lambda e: e.tensor_tensor(tmp3[:], M1[:], slot[:], ALU.mult), r=[b_M])
            V(lambda e: e.reduce_sum(s1f[:], tmp3[:], axis=AX.X))
            V(lambda e: e.tensor_tensor(tmp3[:], M2[:], slot[:], ALU.mult))
            V(lambda e: e.reduce_sum(s2f[:], tmp3[:], axis=AX.X))
            V(lambda e: e.tensor_tensor(tmp3[:], M1[:], gates[:], ALU.mult), r=[b_M])
            V(lambda e: e.reduce_sum(w1[:], tmp3[:], axis=AX.X), w=[b_idx])
            V(lambda e: e.tensor_tensor(tmp3[:], M2[:], gates[:], ALU.mult), r=[b_M])
            V(lambda e: e.reduce_sum(w2[:], tmp3[:], axis=AX.X), w=[b_idx])
            V(lambda e: e.tensor_copy(s1i[:], s1f[:]), w=[b_idx])
            V(lambda e: e.tensor_copy(s2i[:], s2f[:]), w=[b_idx])
            b_io = fw.buf("io")
            fw.op("pool", lambda e: e.iota(tvi[:], [[1, NT]], base=0, channel_multiplier=0), writes=[b_io])
            fw.op("pool", lambda e: e.iota(csti[:], [[128, NHS]], base=0, channel_multiplier=1), writes=[b_io])
            V(lambda e: e.tensor_copy(tv[:], tvi[:]), r=[b_io])
            V(lambda e: e.tensor_copy(cst[:], csti[:]), r=[b_io])
            V(lambda e: e.memset(et[:], 0.0))
            for ee in range(NE - 1):
                V(lambda e: e.scalar_tensor_tensor(et[:], tv[:], sml[:, 17 + ee:18 + ee], et[:], ALU.is_ge, ALU.add))
            V(lambda e: e.tensor_scalar(et[:], et[:], float(NHS * 128), None, ALU.mult))
            V(lambda e: e.tensor_tensor(widxf[:], et[:].unsqueeze(2).to_broadcast([128, NT, NHS]),
                                        cst[:].unsqueeze(1).to_broadcast([128, NT, NHS]), ALU.add))
            V(lambda e: e.tensor_copy(widx[:].rearrange("p (t h) -> p t h", h=NHS), widxf[:]), w=[b_idx])

            for c in range(32):
                fw.idma(XS[:, :], IOA(ap=s1i[:, c:c + 1], axis=0), xn_all[:, c, :], None,
                        reads=[b_xn[c], b_idx], writes=[b_XS], dsem=d_XS)
                fw.idma(XS[:, :], IOA(ap=s2i[:, c:c + 1], axis=0), xn_all[:, c, :], None,
                        reads=[b_xn[c], b_idx], writes=[b_XS], dsem=d_XS)
            fw.barrier()
            esA.close()

            esD = ExitStack()
            NSB = 3
            d_sl = [fw.dsem("ssl%d" % i) for i in range(NSB)]
            slab = [S(esD, "slab%d" % i, [128, 3 * 4096], BF16) for i in range(NSB)]
            b_sl = [fw.buf("ssl%d" % i, d_sl[i]) for i in range(NSB)]
            slg = [slab[i][:, 0:4096].rearrange("p (k n) -> p k n", k=8) for i in range(NSB)]
            slu = [slab[i][:, 4096:8192].rearrange("p (k n) -> p k n", k=8) for i in range(NSB)]
            sld = [slab[i][:, 8192:12288].rearrange("p (f n) -> p f n", f=4) for i in range(NSB)]
            sg = [S(esD, "ssg%d" % i, [128, 512], F32) for i in range(2)]; b_sg = fw.bufs("ssg", 2)
            hid = [S(esD, "shid%d" % i, [128, 4, 512], BF16) for i in range(2)]
            b_hid = [fw.bufs("shid%d_" % i, 4) for i in range(2)]
            d_xsl = [fw.dsem("xsl0"), fw.dsem("xsl1")]
            xsl = [S(esD, "xsl%d" % i, [128, 4, D], BF16) for i in range(2)]
            b_xsl = [fw.buf("xsl%d" % i, d_xsl[i]) for i in range(2)]
            xsT = [S(esD, "xsT%d" % i, [128, 8, 512], BF16) for i in range(2)]
            b_xsT = [fw.bufs("xsT%d_" % i, 4) for i in range(2)]
            accY = [S(esD, "accY%d" % i, [128, 4, D], F32) for i in range(2)]
            b_accY = [fw.bufs("accY%d_" % i, 4) for i in range(2)]
            idb4 = S(esD, "idb4", [128, 128], BF16); b_idb4 = fw.buf("idb4")
            fw.op("dve", lambda e: e.tensor_copy(idb4[:], ident[:]), reads=[b_ident], writes=[b_idb4])
            psb7 = ps[7][:].bitcast(BF16)
            b_wall = [b_wf[1 + e_] for e_ in range(NE)]

            def load_tile(t):
                if t < NT:
                    fw.dma("sp", xsl[t % 2][:], XS[t * 512:(t + 1) * 512, :].rearrange("(c p) d -> p c d", p=128),
                           reads=[b_XS], writes=[b_xsl[t % 2]])

            def prep_tile(t):
                if t >= NT:
                    return
                for c in range(4):
                    for k in range(8):
                        fw.op("pe", lambda e: e.transpose(psb7[:, k * 128:(k + 1) * 128], xsl[t % 2][:, c, k * 128:(k + 1) * 128], idb4[:]),
                              reads=[b_xsl[t % 2], b_idb4], writes=[b_ps[7]])
                    fw.op("act", lambda e: e.copy(xsT[t % 2][:, :, c * 128:(c + 1) * 128],
                                                  psb7[:, :].rearrange("p (k q) -> p k q", k=8)),
                          reads=[b_ps[7]], writes=[b_xsT[t % 2][c]])

            def load_slab(n):
                if n < NT * NHS:
                    fw.idma(slab[n % NSB][:, :], None, wgud[:, :], IOA(ap=widx[:, n:n + 1], axis=0),
                            reads=[b_idx] + b_wall, writes=[b_sl[n % NSB]], dsem=d_sl[n % NSB])

            load_tile(0)
            load_tile(1)
            prep_tile(0)
            load_slab(0)
            load_slab(1)
            ndd = 0
            nhid = 0
            for t in range(NT):
                xT, b_xT = xsT[t % 2], b_xsT[t % 2]
                aY, b_aY = accY[t % 2], b_accY[t % 2]
                for hs in range(NHS):
                    n = t * NHS + hs
                    si = n % NSB
                    load_slab(n + 2)
                    hi = nhid % 2
                    nhid += 1
                    for f in range(4):
                        pg, pu = (0, 1) if f % 2 == 0 else (2, 3)
                        for k in range(8):
                            fw.op("pe", lambda e: e.matmul(ps[pg][:], lhsT=slg[si][:, k, f * 128:(f + 1) * 128],
                                                           rhs=xT[:, k, :], start=(k == 0), stop=(k == 7)),
                                  reads=[b_sl[si]] + b_xT, writes=[b_ps[pg]])
                        for k in range(8):
                            fw.op("pe", lambda e: e.matmul(ps[pu][:], lhsT=slu[si][:, k, f * 128:(f + 1) * 128],
                                                           rhs=xT[:, k, :], start=(k == 0), stop=(k == 7)),
                                  reads=[b_sl[si]] + b_xT, writes=[b_ps[pu]])
                        fw.op("act", lambda e: e.activation(sg[f % 2][:], ps[pg][:], AF.Silu),
                              reads=[b_ps[pg]], writes=[b_sg[f % 2]])
                        fw.op("dve", lambda e: e.tensor_tensor(hid[hi][:, f, :], sg[f % 2][:], ps[pu][:], ALU.mult),
                              reads=[b_sg[f % 2], b_ps[pu]], writes=[b_hid[hi][f]])
                    for c in range(4):
                        for s2_ in range(2):
                            pd = 4 + ndd % 3
                            ndd += 1
                            for f in range(4):
                                fw.op("pe", lambda e: e.matmul(ps[pd][:], lhsT=hid[hi][:, f, c * 128:(c + 1) * 128],
                                                               rhs=sld[si][:, f, s2_ * 512:(s2_ + 1) * 512],
                                                               start=(f == 0), stop=(f == 3)),
                                      reads=[b_hid[hi][f], b_sl[si]], writes=[b_ps[pd]])
                            a_ap = aY[:, c, s2_ * 512:(s2_ + 1) * 512]
                            if hs == 0:
                                fw.op("dve", lambda e: e.tensor_copy(a_ap, ps[pd][:]), reads=[b_ps[pd]], writes=[b_aY[c]])
                            else:
                                fw.op("dve", lambda e: e.tensor_tensor(a_ap, ps[pd][:], a_ap, ALU.add),
                                      reads=[b_ps[pd], b_aY[c]], writes=[b_aY[c]])
                    if hs == 3:
                        prep_tile(t + 1)
                        load_tile(t + 2)
                for c in range(4):
                    fw.dma("sp", YS[t * 512 + c * 128:t * 512 + (c + 1) * 128, :], aY[:, c, :],
                           reads=[b_aY[c]], writes=[b_YS])
            fw.barrier()
            esD.close()

            d_y = [fw.dsem("ya0"), fw.dsem("ya1")]
            ya = [S(es, "ya%d" % i, [128, D], F32) for i in range(2)]
            yb = [S(es, "yb%d" % i, [128, D], F32) for i in range(2)]
            b_y = [fw.buf("y%d" % i, d_y[i]) for i in range(2)]
            d_hf = [fw.dsem("hf0"), fw.dsem("hf1")]
            hf_ = [S(es, "hf%d" % i, [128, D], F32) for i in range(2)]
            b_hf = [fw.buf("hf%d" % i, d_hf[i]) for i in range(2)]
            ft2 = [S(es, "ft%d" % i, [128, D], F32) for i in range(2)]; b_ft2 = fw.bufs("ft", 2)

            def load_fin(c):
                if c >= 32:
                    return
                sl_ = c % 2
                fw.idma(ya[sl_][:, :], None, YS[:, :], IOA(ap=s1i[:, c:c + 1], axis=0), reads=[b_YS, b_idx], writes=[b_y[sl_]])
                fw.idma(yb[sl_][:, :], None, YS[:, :], IOA(ap=s2i[:, c:c + 1], axis=0), reads=[b_YS, b_idx], writes=[b_y[sl_]])
                fw.dma("sp", hf_[sl_][:], hB[c * 128:(c + 1) * 128, :], reads=[b_hB], writes=[b_hf[sl_]])

            load_fin(0)
            for c in range(32):
                sl_ = c % 2
                load_fin(c + 1)
                ft, b_ft = ft2[sl_], b_ft2[sl_]
                ss, b_ss = ss2[sl_], b_ss2[sl_]
                fw.op("dve", lambda e: e.tensor_scalar(ft[:], ya[sl_][:], w1[:, c:c + 1], None, ALU.mult),
                      reads=[b_y[sl_], b_idx], writes=[b_ft])
                fw.op("dve", lambda e: e.scalar_tensor_tensor(ft[:], yb[sl_][:], w2[:, c:c + 1], ft[:], ALU.mult, ALU.add),
                      reads=[b_y[sl_], b_idx, b_ft], writes=[b_ft])
                fw.op("dve", lambda e: e.tensor_tensor(ft[:], ft[:], g2r[:], ALU.mult), reads=[b_ft, b_g2r], writes=[b_ft])
                fw.op("dve", lambda e: e.tensor_tensor(ft[:], ft[:], hf_[sl_][:], ALU.add), reads=[b_ft, b_hf[sl_]], writes=[b_ft])
                fw.op("dve", lambda e: e.memset(ss[:, 0:1], 0.0), writes=[b_ss])
                fw.op("act", lambda e: e.activation(junk[:], ft[:], AF.Square, accum_out=ss[:, 0:1]),
                      reads=[b_ft], writes=[b_ss])
                fw.op("act", lambda e: e.activation(ss[:, 1:2], ss[:, 0:1], AF.Sqrt, bias=eps_t[:, 0:1], scale=1.0 / D),
                      reads=[b_ss, b_eps], writes=[b_ss])
                fw.op("dve", lambda e: e.reciprocal(ss[:, 2:3], ss[:, 1:2]), reads=[b_ss], writes=[b_ss])
                fw.op("dve", lambda e: e.scalar_tensor_tensor(ft[:], ft[:], ss[:, 2:3], fgb[:], ALU.mult, ALU.mult),
                      reads=[b_ft, b_ss, b_fgb], writes=[b_ft])
                fw.dma("sp", out_d[c * 128:(c + 1) * 128, :], ft[:], reads=[b_ft], writes=[b_out])
            fw.barrier()

    if stop_after >= 4:
        moe_sparse_phase()

    fw.finish("sp")
    return nc


def _rope_tables(hf):
    pos = np.concatenate([
        hf * OWN + np.arange(OWN),
        hf * OWN - 128 + np.arange(128),
        hf * OWN + OWN + np.arange(128),
    ])
    valid = (pos >= 0) & (pos < SEQ)
    pos = np.clip(pos, 0, SEQ - 1)
    row = (pos // 64).astype(np.float32)
    col = (pos % 64).astype(np.float32)
    inv = (np.float32(10000.0) ** (-np.arange(16, dtype=np.float32) / np.float32(16))).astype(np.float32)
    cos_t = np.ones((64, NCH * 128), np.float32)
    sin_t = np.zeros((64, NCH * 128), np.float32)
    n = pos.shape[0]
    for d in range(64):
        p = row if d < 32 else col
        jj = d % 16
        sign = -1.0 if (d % 32) < 16 else 1.0
        ang = (p * inv[jj]).astype(np.float32)
        cos_t[d, :n] = np.cos(ang).astype(np.float32)
        sin_t[d, :n] = (sign * np.sin(ang)).astype(np.float32)
    del valid
    return cos_t, sin_t


def _masks(hf):
    kk = np.arange(128)[:, None]
    q = np.arange(128)[None, :]
    prev = (kk >= q).astype(np.float32)
    nxt = (kk <= q).astype(np.float32)
    m = np.zeros((128, 4, 128), np.float32)
    m[:, 0] = prev
    m[:, 1] = nxt
    m[:, 2] = prev if hf == 1 else 0.0
    m[:, 3] = nxt if hf == 0 else 0.0
    return m


def make_in_maps(inp):
    f = lambda a: np.ascontiguousarray(np.asarray(a, dtype=np.float32))
    x = f(inp["x"]); c = f(inp["c"]); ctx = f(inp["ctx"]); c_ctx = f(inp["c_ctx"])
    shared = {
        "ada_w": f(inp["ada_w"]),
        "ada_bT": f(np.asarray(inp["ada_b"]).reshape(2, 48, 128).transpose(2, 0, 1)),
        "ada_b": f(inp["ada_b"]),
        "norm_gT": f(np.asarray(inp["norm_g"]).reshape(2, 2, 8, 128).transpose(3, 0, 1, 2).reshape(128, 32)),
        "final_g": f(inp["final_g"]),
        "a_w_in": f(inp["a_w_in"][0]), "a_v_g": f(inp["a_v_g"][0]), "a_v_b": f(inp["a_v_b"][0]),
        "a_w_s": f(inp["a_w_s"][0]), "a_b_sT": f(np.asarray(inp["a_b_s"][0]).T),
        "a_w_out": f(inp["a_w_out"][0]),
        "b_w_qkv": f(inp["b_w_qkv"][0]), "b_sink": f(inp["b_sink"][0]), "b_w_out": f(inp["b_w_out"][0]),
        "ffn_w_gate": f(inp["ffn_w_gate"][0]), "ffn_w_up": f(inp["ffn_w_up"][0]), "ffn_w_down": f(inp["ffn_w_down"][0]),
        "moe_w_router": f(inp["moe_w_router"][0]),
        "moe_w_gate": f(inp["moe_w_gate"][0]), "moe_w_up": f(inp["moe_w_up"][0]), "moe_w_down": f(inp["moe_w_down"][0]),
        "ident": np.eye(128, dtype=np.float32),
        "tri": np.triu(np.ones((128, 128), np.float32), 1),
        "norm_g_raw": f(inp["norm_g"]),
    }
    maps = []
    for r in range(8):
        b, hf = r // 2, r % 2
        x_loc = np.zeros((OWN + 256, D), np.float32)
        x_loc[:OWN] = x[b, hf * OWN:(hf + 1) * OWN]
        if hf == 1:
            x_loc[OWN:OWN + 128] = x[b, OWN - 128:OWN]
        else:
            x_loc[OWN + 128:OWN + 256] = x[b, OWN:OWN + 128]
        c2 = np.stack([c[b], c_ctx], axis=-1).reshape(8, 128, 2).transpose(1, 0, 2).reshape(128, 16)
        cos_t, sin_t = _rope_tables(hf)
        m = dict(shared)
        m.update({"x_loc": x_loc, "ctx_b": f(ctx[b]), "c2": f(c2), "cos_t": cos_t, "sin_t": sin_t,
                  "masks": _masks(hf)})
        maps.append(m)
    return maps


_NC_CACHE = {}


def kernel(**inputs):
    if "nc" not in _NC_CACHE:
        _NC_CACHE["nc"] = build()
    nc = _NC_CACHE["nc"]
    maps = make_in_maps(inputs)
    res = run_bass_kernel_spmd(nc, maps, core_ids=list(range(8)))
    out = np.zeros((4, SEQ, D), np.float32)
    for r in range(8):
        b, hf = r // 2, r % 2
        out[b, hf * OWN:(hf + 1) * OWN] = res.results[r]["out"]
    return out
```
